# Optimizing a Trainium2 kernel written in Bass

```python
import math
import jax, jax.numpy as jnp
from jax import lax
import numpy as np


D_MODEL = 2048
BATCH = 4
SEQ = 2048
DEPTH = 1

HEAD_DIM = 128
N_NSA_HEADS = D_MODEL // (2 * HEAD_DIM)
N_NSA_KV = max(1, N_NSA_HEADS // 4)
N_FOX_HEADS = D_MODEL // (2 * HEAD_DIM)
D_NSA = N_NSA_HEADS * HEAD_DIM
D_FOX = N_FOX_HEADS * HEAD_DIM
D_MIX = D_NSA + D_FOX
D_KV = N_NSA_KV * HEAD_DIM
IN_SIZES = (D_NSA, D_KV, D_KV, D_KV, D_KV, D_KV, D_KV, 3 * N_NSA_HEADS, D_FOX, D_FOX, D_FOX, N_FOX_HEADS)
D_IN = sum(IN_SIZES)
IN_SPLITS = tuple(int(v) for v in np.cumsum(IN_SIZES)[:-1])

CMP_LEN = 32
CMP_STRIDE = 16
CMP_HIDDEN = 256
SLC_BLOCK = 64
SLC_TOPK = 16
WINDOW = 512
Q_BLOCK = 128
SLC_Q_BLOCK = 64
N_BUCKETS = 32
MAX_DISTANCE = 128
N_EXPERTS = 32
TOP_K = 4
EXPERT_FF = D_MODEL
SWIGLU_ALPHA = 1.702
SWIGLU_LIMIT = 7.0
MOE_BLOCK = 256
SCALE = HEAD_DIM ** -0.5
NEG_INF = -1e30
FORCED_SCORE = 1e6
EPS = 1e-6

kernel_name = "hymba_nsa_fox_moe_block"


def rms_norm(x, g):
    xf = x.astype(jnp.float32)
    y = xf * lax.rsqrt(jnp.mean(xf * xf, axis=-1, keepdims=True) + EPS)
    return (y * g.astype(jnp.float32)).astype(x.dtype)


def masked_softmax(logits, mask):
    logits = jnp.where(mask, logits.astype(jnp.float32), NEG_INF)
    return jax.nn.softmax(logits, axis=-1) * mask


def t5_bucket(dist):
    n = jnp.maximum(dist, 0)
    max_exact = N_BUCKETS // 2
    nf = jnp.maximum(n, 1).astype(jnp.float32)
    large = max_exact + (jnp.log(nf / max_exact) / math.log(MAX_DISTANCE / max_exact)
                         * (N_BUCKETS - max_exact)).astype(jnp.int32)
    large = jnp.minimum(large, N_BUCKETS - 1)
    return jnp.where(n < max_exact, n, large)


def nsa_compress(k, pos, w1, w2):
    B, S, G, dh = k.shape
    n_cmp = (S - CMP_LEN) // CMP_STRIDE + 1
    idx = jnp.arange(n_cmp)[:, None] * CMP_STRIDE + jnp.arange(CMP_LEN)[None, :]
    blocks = k[:, idx] + pos[:, None, :]
    flat = blocks.transpose(0, 1, 3, 2, 4).reshape(B, n_cmp, G, CMP_LEN * dh)
    return jax.nn.gelu(flat @ w1) @ w2


def nsa_compressed_branch(qg, kc, vc, rel_bias):
    B, S, G, R, _ = qg.shape
    n_cmp = kc.shape[1]
    logits = jnp.einsum('bsgrd,bngd->bgrsn', qg, kc).astype(jnp.float32) * SCALE
    dist = jnp.arange(S)[:, None] - (jnp.arange(n_cmp) * CMP_STRIDE + CMP_LEN - 1)[None, :]
    bias = rel_bias[t5_bucket(dist)].reshape(S, n_cmp, G, R).transpose(2, 3, 0, 1)
    p = masked_softmax(logits + bias, dist >= 0)
    o = jnp.einsum('bgrsn,bngd->bsgrd', p.astype(vc.dtype), vc)
    return o, p


def nsa_select_blocks(p_cmp, S):
    n_cmp = p_cmp.shape[-1]
    n_slc = S // SLC_BLOCK
    k_sel = min(SLC_TOPK, n_slc)
    cmp_start = jnp.arange(n_cmp) * CMP_STRIDE
    slc_start = jnp.arange(n_slc) * SLC_BLOCK
    overlap = ((cmp_start[:, None] < slc_start[None, :] + SLC_BLOCK)
               & (cmp_start[:, None] + CMP_LEN > slc_start[None, :])).astype(jnp.float32)
    imp = jnp.einsum('bgrsn,nj->bgsj', p_cmp, overlap)
    t = jnp.arange(S)[:, None]
    j = jnp.arange(n_slc)[None, :]
    cur = t // SLC_BLOCK
    forced = (j == 0) | (j == cur) | (j == cur - 1)
    valid = j <= cur
    imp = jnp.where(forced, FORCED_SCORE, jnp.where(valid, imp, -FORCED_SCORE))
    _, sel = lax.top_k(imp, k_sel)
    return sel


def nsa_selected_branch(qg, k, v, sel, rel_bias):
    B, S, G, R, dh = qg.shape
    n_tok = sel.shape[-1] * SLC_BLOCK
    k_t = k.transpose(0, 2, 1, 3)
    v_t = v.transpose(0, 2, 1, 3)
    table_gr = rel_bias.reshape(N_BUCKETS, G, R).transpose(1, 0, 2)
    gather = jax.vmap(jax.vmap(lambda src, idx: src[idx]))
    g_idx = jnp.arange(G)[None, :, None, None]

    def chunk(ci):
        q0 = ci * SLC_Q_BLOCK
        qc = lax.dynamic_slice_in_dim(qg, q0, SLC_Q_BLOCK, axis=1)
        sc = lax.dynamic_slice_in_dim(sel, q0, SLC_Q_BLOCK, axis=2)
        tok = (sc[..., None] * SLC_BLOCK + jnp.arange(SLC_BLOCK)).reshape(B, G, SLC_Q_BLOCK * n_tok)
        kk = gather(k_t, tok).reshape(B, G, SLC_Q_BLOCK, n_tok, dh)
        vv = gather(v_t, tok).reshape(B, G, SLC_Q_BLOCK, n_tok, dh)
        logits = jnp.einsum('bqgrd,bgqkd->bgrqk', qc, kk).astype(jnp.float32) * SCALE
        dist = (q0 + jnp.arange(SLC_Q_BLOCK))[:, None] - tok.reshape(B, G, SLC_Q_BLOCK, n_tok)
        bias = table_gr[g_idx, t5_bucket(dist)]
        p = masked_softmax(logits + jnp.moveaxis(bias, -1, 2), (dist >= 0)[:, :, None])
        return jnp.einsum('bgrqk,bgqkd->bqgrd', p.astype(vv.dtype), vv)

    out = lax.map(chunk, jnp.arange(S // SLC_Q_BLOCK))
    return jnp.moveaxis(out, 0, 1).reshape(B, S, G * R, dh)


def nsa_window_branch(qg, kw, vw, rel_bias):
    B, S, G, R, dh = qg.shape
    n_kw = WINDOW + Q_BLOCK
    kp = jnp.pad(kw, ((0, 0), (WINDOW, 0), (0, 0), (0, 0)))
    vp = jnp.pad(vw, ((0, 0), (WINDOW, 0), (0, 0), (0, 0)))
    rel = jnp.arange(Q_BLOCK)[:, None] + WINDOW - jnp.arange(n_kw)[None, :]
    bias = rel_bias[t5_bucket(rel)].reshape(Q_BLOCK, n_kw, G, R).transpose(2, 3, 0, 1)
    band = (rel >= 0) & (rel < WINDOW)

    def chunk(ci):
        q0 = ci * Q_BLOCK
        qc = lax.dynamic_slice_in_dim(qg, q0, Q_BLOCK, axis=1)
        kc = lax.dynamic_slice_in_dim(kp, q0, n_kw, axis=1)
        vc = lax.dynamic_slice_in_dim(vp, q0, n_kw, axis=1)
        logits = jnp.einsum('bqgrd,bkgd->bgrqk', qc, kc).astype(jnp.float32) * SCALE + bias
        mask = band & (q0 - WINDOW + jnp.arange(n_kw) >= 0)[None, :]
        p = masked_softmax(logits, mask)
        return jnp.einsum('bgrqk,bkgd->bqgrd', p.astype(vc.dtype), vc)

    out = lax.map(chunk, jnp.arange(S // Q_BLOCK))
    return jnp.moveaxis(out, 0, 1).reshape(B, S, G * R, dh)


def forgetting_attention(q, k, v, f_logit):
    B, S, H, dh = q.shape
    cum = jnp.cumsum(jax.nn.log_sigmoid(f_logit.astype(jnp.float32)), axis=1).transpose(0, 2, 1)
    kpos = jnp.arange(S)

    def chunk(ci):
        q0 = ci * Q_BLOCK
        qc = lax.dynamic_slice_in_dim(q, q0, Q_BLOCK, axis=1)
        cq = lax.dynamic_slice_in_dim(cum, q0, Q_BLOCK, axis=2)
        logits = (jnp.einsum('bqhd,bkhd->bhqk', qc, k).astype(jnp.float32) * SCALE
                  + cq[..., None] - cum[:, :, None, :])
        mask = (q0 + jnp.arange(Q_BLOCK))[:, None] >= kpos[None, :]
        p = masked_softmax(logits, mask)
        return jnp.einsum('bhqk,bkhd->bqhd', p.astype(v.dtype), v)

    out = lax.map(chunk, jnp.arange(S // Q_BLOCK))
    return jnp.moveaxis(out, 0, 1).reshape(B, S, H, dh)


def token_mixers(h, rel_bias, w_in, b_nsa_gate, nsa_q_norm, nsa_k_norm, cmp_pos_k, cmp_pos_v,
                 cmp_k_w1, cmp_k_w2, cmp_v_w1, cmp_v_w2, fox_q_norm, fox_k_norm, fox_forget_bias, out_norm):
    B, S, _ = h.shape
    G, R = N_NSA_KV, N_NSA_HEADS // N_NSA_KV
    proj = h @ w_in
    q_n, k_c, v_c, k_s, v_s, k_w, v_w, g_n, q_f, k_f, v_f, f_f = jnp.split(proj, IN_SPLITS, axis=-1)
    hd = lambda t, n: t.reshape(B, S, n, HEAD_DIM)
    qg = rms_norm(hd(q_n, N_NSA_HEADS), nsa_q_norm).reshape(B, S, G, R, HEAD_DIM)
    kc = rms_norm(nsa_compress(hd(k_c, G), cmp_pos_k, cmp_k_w1, cmp_k_w2), nsa_k_norm[0])
    vc = nsa_compress(hd(v_c, G), cmp_pos_v, cmp_v_w1, cmp_v_w2)
    o_cmp, p_cmp = nsa_compressed_branch(qg, kc, vc, rel_bias)
    o_cmp = o_cmp.reshape(B, S, N_NSA_HEADS, HEAD_DIM)
    sel = nsa_select_blocks(p_cmp, S)
    o_slc = nsa_selected_branch(qg, rms_norm(hd(k_s, G), nsa_k_norm[1]), hd(v_s, G), sel, rel_bias)
    o_win = nsa_window_branch(qg, rms_norm(hd(k_w, G), nsa_k_norm[2]), hd(v_w, G), rel_bias)
    gates = jax.nn.sigmoid(g_n + b_nsa_gate).reshape(B, S, 3, N_NSA_HEADS, 1)
    o_nsa = gates[:, :, 0] * o_cmp + gates[:, :, 1] * o_slc + gates[:, :, 2] * o_win
    o_fox = forgetting_attention(rms_norm(hd(q_f, N_FOX_HEADS), fox_q_norm),
                                 rms_norm(hd(k_f, N_FOX_HEADS), fox_k_norm),
                                 hd(v_f, N_FOX_HEADS), f_f + fox_forget_bias)
    o_nsa = rms_norm(o_nsa.reshape(B, S, D_NSA), out_norm[:D_NSA])
    o_fox = rms_norm(o_fox.reshape(B, S, D_FOX), out_norm[D_NSA:])
    return jnp.concatenate([o_nsa, o_fox], axis=-1)


def moe_ffn(h, router_w, router_b, w_up, b_up, w_down, b_down):
    B, S, D = h.shape
    T = B * S
    TK = T * TOP_K
    xs = h.reshape(T, D)
    logits = (xs @ router_w + router_b).astype(jnp.float32)
    top_v, top_i = lax.top_k(logits, TOP_K)
    gate = jax.nn.softmax(top_v, axis=-1).astype(xs.dtype)
    flat_e = top_i.reshape(-1)
    flat_tok = jnp.arange(TK) // TOP_K
    flat_w = gate.reshape(-1)
    order = jnp.argsort(flat_e)
    se, stok, sw = flat_e[order], flat_tok[order], flat_w[order]
    counts = jnp.zeros((N_EXPERTS,), jnp.int32).at[flat_e].add(1)
    pcounts = (counts + MOE_BLOCK - 1) // MOE_BLOCK * MOE_BLOCK
    off = jnp.cumsum(counts) - counts
    pend = jnp.cumsum(pcounts)
    poff = pend - pcounts
    dest = poff[se] + (jnp.arange(TK) - off[se])
    n_blocks = -(-TK // MOE_BLOCK) + N_EXPERTS
    n_rows = n_blocks * MOE_BLOCK
    row_tok = jnp.full((n_rows,), T, jnp.int32).at[dest].set(stok)
    row_w = jnp.zeros((n_rows,), xs.dtype).at[dest].set(sw)
    block_e = jnp.minimum(jnp.sum((jnp.arange(n_blocks) * MOE_BLOCK)[:, None] >= pend[None, :], axis=1),
                          N_EXPERTS - 1)
    x_pad = jnp.concatenate([xs, jnp.zeros((1, D), xs.dtype)], axis=0)

    def expert_block(args):
        tok, w, e = args
        hu = x_pad[tok] @ w_up[e] + b_up[e]
        glu = jnp.minimum(hu[:, :EXPERT_FF], SWIGLU_LIMIT)
        lin = jnp.clip(hu[:, EXPERT_FF:], -SWIGLU_LIMIT, SWIGLU_LIMIT)
        act = glu * jax.nn.sigmoid(SWIGLU_ALPHA * glu) * (lin + 1)
        return (act @ w_down[e] + b_down[e]) * w[:, None]

    ys = lax.map(expert_block, (row_tok.reshape(n_blocks, MOE_BLOCK), row_w.reshape(n_blocks, MOE_BLOCK), block_e))
    out = jnp.zeros((T + 1, D), ys.dtype).at[row_tok].add(ys.reshape(n_rows, D))[:T]
    return out.reshape(B, S, D)


def hybrid_layer(x, c, rel_bias, mod_w, mod_b, norm_attn, norm_moe, w_in, b_nsa_gate, nsa_q_norm, nsa_k_norm,
                 cmp_pos_k, cmp_pos_v, cmp_k_w1, cmp_k_w2, cmp_v_w1, cmp_v_w2, fox_q_norm, fox_k_norm,
                 fox_forget_bias, out_norm, w_out, router_w, router_b, exp_w_up, exp_b_up, exp_w_down, exp_b_down):
    mod = jax.nn.silu(c) @ mod_w + mod_b
    shift_a, scale_a, gate_a, shift_m, scale_m, gate_m = jnp.split(mod[:, None, :], 6, axis=-1)
    h = rms_norm(x, norm_attn) * (1 + scale_a) + shift_a
    mix = token_mixers(h, rel_bias, w_in, b_nsa_gate, nsa_q_norm, nsa_k_norm, cmp_pos_k, cmp_pos_v,
                       cmp_k_w1, cmp_k_w2, cmp_v_w1, cmp_v_w2, fox_q_norm, fox_k_norm, fox_forget_bias, out_norm)
    x = x + gate_a * (mix @ w_out)
    h = rms_norm(x, norm_moe) * (1 + scale_m) + shift_m
    x = x + gate_m * moe_ffn(h, router_w, router_b, exp_w_up, exp_b_up, exp_w_down, exp_b_down)
    return x


def setup_inputs(seed: int = 0) -> dict:
    key = jax.random.key(seed)
    ks = jax.random.split(key, 32)
    D, L, E, F = D_MODEL, DEPTH, N_EXPERTS, EXPERT_FF

    def nrm(k, shape, s):
        return jax.random.normal(k, shape, jnp.float32) * s

    return {
        "x": nrm(ks[0], (BATCH, SEQ, D), 1.0),
        "c": nrm(ks[1], (BATCH, D), 1.0),
        "rel_bias": nrm(ks[2], (N_BUCKETS, N_NSA_HEADS), 0.5),
        "mod_w": nrm(ks[3], (L, D, 6 * D), D ** -0.5),
        "mod_b": nrm(ks[4], (L, 6 * D), 0.02),
        "norm_attn": 1.0 + nrm(ks[5], (L, D), 0.02),
        "norm_moe": 1.0 + nrm(ks[6], (L, D), 0.02),
        "w_in": nrm(ks[7], (L, D, D_IN), D ** -0.5),
        "b_nsa_gate": nrm(ks[8], (L, 3 * N_NSA_HEADS), 0.1),
        "nsa_q_norm": 1.0 + nrm(ks[9], (L, HEAD_DIM), 0.02),
        "nsa_k_norm": 1.0 + nrm(ks[10], (L, 3, HEAD_DIM), 0.02),
        "cmp_pos_k": nrm(ks[11], (L, CMP_LEN, HEAD_DIM), 0.1),
        "cmp_pos_v": nrm(ks[12], (L, CMP_LEN, HEAD_DIM), 0.1),
        "cmp_k_w1": nrm(ks[13], (L, CMP_LEN * HEAD_DIM, CMP_HIDDEN), (CMP_LEN * HEAD_DIM) ** -0.5),
        "cmp_k_w2": nrm(ks[14], (L, CMP_HIDDEN, HEAD_DIM), CMP_HIDDEN ** -0.5),
        "cmp_v_w1": nrm(ks[15], (L, CMP_LEN * HEAD_DIM, CMP_HIDDEN), (CMP_LEN * HEAD_DIM) ** -0.5),
        "cmp_v_w2": nrm(ks[16], (L, CMP_HIDDEN, HEAD_DIM), CMP_HIDDEN ** -0.5),
        "fox_q_norm": 1.0 + nrm(ks[17], (L, HEAD_DIM), 0.02),
        "fox_k_norm": 1.0 + nrm(ks[18], (L, HEAD_DIM), 0.02),
        "fox_forget_bias": 2.0 + nrm(ks[19], (L, N_FOX_HEADS), 0.5),
        "out_norm": 1.0 + nrm(ks[20], (L, D_MIX), 0.02),
        "w_out": nrm(ks[21], (L, D_MIX, D), D_MIX ** -0.5),
        "router_w": nrm(ks[22], (L, D, E), D ** -0.5),
        "router_b": nrm(ks[23], (L, E), 0.01),
        "exp_w_up": nrm(ks[24], (L, E, D, 2 * F), D ** -0.5),
        "exp_b_up": nrm(ks[25], (L, E, 2 * F), 0.01),
        "exp_w_down": nrm(ks[26], (L, E, F, D), F ** -0.5),
        "exp_b_down": nrm(ks[27], (L, E, D), 0.01),
    }


def reference(x, c, rel_bias, mod_w, mod_b, norm_attn, norm_moe, w_in, b_nsa_gate, nsa_q_norm, nsa_k_norm,
              cmp_pos_k, cmp_pos_v, cmp_k_w1, cmp_k_w2, cmp_v_w1, cmp_v_w2, fox_q_norm, fox_k_norm,
              fox_forget_bias, out_norm, w_out, router_w, router_b, exp_w_up, exp_b_up, exp_w_down, exp_b_down):
    for l in range(DEPTH):
        x = hybrid_layer(x, c, rel_bias, mod_w[l], mod_b[l], norm_attn[l], norm_moe[l], w_in[l], b_nsa_gate[l],
                         nsa_q_norm[l], nsa_k_norm[l], cmp_pos_k[l], cmp_pos_v[l], cmp_k_w1[l], cmp_k_w2[l],
                         cmp_v_w1[l], cmp_v_w2[l], fox_q_norm[l], fox_k_norm[l], fox_forget_bias[l], out_norm[l],
                         w_out[l], router_w[l], router_b[l], exp_w_up[l], exp_b_up[l], exp_w_down[l], exp_b_down[l])
    return x
```

```python
import math
import numpy as np
import concourse.bass as bass
import concourse.mybir as mybir
from concourse.bass_utils import run_bass_kernel_spmd

F32 = mybir.dt.float32
BF16 = mybir.dt.bfloat16
ALU = mybir.AluOpType
AF = mybir.ActivationFunctionType
AX = mybir.AxisListType
PE, ACT, DVE, POOL, SP = "pe", "act", "dve", "pool", "sp"


def _tname(ap):
    t = getattr(ap, "tensor", None)
    if t is None:
        t = ap
    return t.name


class Op:
    __slots__ = ("eng", "fn", "args", "kw", "reads", "writes", "dma", "deps", "sig", "semval", "grp", "bar")

    def __init__(self, eng, fn, args, kw, reads, writes, dma=False, grp=None):
        self.eng = eng
        self.fn = fn
        self.args = args
        self.kw = kw
        self.reads = reads
        self.writes = writes
        self.dma = dma
        self.deps = []
        self.sig = False
        self.semval = None
        self.grp = grp
        self.bar = False


class K:
    def __init__(self, nc, same_engine_sync=True):
        self.nc = nc
        self.ops = []
        self.same_engine_sync = same_engine_sync
        self.engs = {PE: nc.tensor, ACT: nc.scalar, DVE: nc.vector, POOL: nc.gpsimd, SP: nc.sync}

    def _rec(self, eng, fn, args, kw, reads, writes, dma=False, grp=None):
        r = set(_tname(a) for a in reads if a is not None and not isinstance(a, (int, float)))
        w = set(_tname(a) for a in writes)
        self.ops.append(Op(eng, fn, args, kw, r, w, dma, grp))

    def barrier(self):
        o = Op(None, None, None, None, set(), set())
        o.bar = True
        self.ops.append(o)

    def mm(self, out, lhsT, rhs, start=True, stop=True):
        self._rec(PE, "matmul", (out, lhsT, rhs), dict(start=start, stop=stop), [lhsT, rhs] + ([] if start else [out]), [out])

    def act(self, out, in_, func, bias=None, scale=None, accum_out=None):
        kw = {}
        reads = [in_]
        if bias is not None:
            kw["bias"] = bias
            reads.append(bias)
        if scale is not None:
            kw["scale"] = scale
            reads.append(scale)
        writes = [out]
        if accum_out is not None:
            kw["accum_out"] = accum_out
            writes.append(accum_out)
        self._rec(ACT, "activation", (out, in_, func), kw, reads, writes)

    def tt(self, out, in0, in1, op, eng=DVE):
        self._rec(eng, "tensor_tensor", (out, in0, in1, op), {}, [in0, in1], [out])

    def ts(self, out, in0, s1, s2, op0, op1=None, eng=DVE):
        kw = {}
        if op1 is not None:
            kw["op1"] = op1
        self._rec(eng, "tensor_scalar", (out, in0, s1, s2, op0), kw, [in0, s1, s2], [out])

    def stt(self, out, in0, scalar, in1, op0, op1):
        self._rec(DVE, "scalar_tensor_tensor", (out, in0, scalar, in1, op0, op1), {}, [in0, scalar, in1], [out])

    def copy(self, out, in_, eng=DVE):
        if eng == ACT:
            self._rec(ACT, "copy", (out, in_), {}, [in_], [out])
        else:
            self._rec(eng, "tensor_copy", (out, in_), {}, [in_], [out])

    def memset(self, out, val, eng=DVE):
        self._rec(eng, "memset", (out, val), {}, [], [out])

    def recip(self, out, in_):
        self._rec(DVE, "reciprocal", (out, in_), {}, [in_], [out])

    def max8(self, out, in_):
        self._rec(DVE, "max", (out, in_), {}, [in_], [out])

    def match_replace(self, out, in_to_replace, in_values, imm):
        self._rec(DVE, "match_replace", (out, in_to_replace, in_values, imm), {}, [in_to_replace, in_values], [out])

    def reduce_sum(self, out, in_, eng=DVE):
        self._rec(eng, "reduce_sum", (out, in_, AX.X), {}, [in_], [out])

    def dma(self, out, in_, eng=SP, grp=None):
        self._rec(eng, "dma_start", (out, in_), {}, [in_], [out], dma=True, grp=grp)

    def emit(self, final_wait_tensors=()):
        nc = self.nc
        ops = self.ops
        last_w = {}
        readers = {}
        last_eng = {}
        dma_since = []
        bar_deps = []
        need_bar = set()
        for i, op in enumerate(ops):
            if op.bar:
                bar_deps = list(last_eng.values()) + dma_since
                dma_since = []
                need_bar = set(self.engs.keys())
                continue
            deps = set()
            for t in op.reads | op.writes:
                for j in last_w.get(t, ()):
                    deps.add(j)
            for t in op.writes:
                for j in readers.get(t, ()):
                    deps.add(j)
            if op.grp is not None:
                deps = {j for j in deps if not (ops[j].grp == op.grp)}
            if op.eng in need_bar:
                deps.update(bar_deps)
                need_bar.discard(op.eng)
            keep = []
            for j in deps:
                oj = ops[j]
                if oj.eng == op.eng and not oj.dma:
                    if op.eng == PE or not self.same_engine_sync:
                        continue
                keep.append(j)
            op.deps = keep
            for j in keep:
                ops[j].sig = True
            for t in op.reads:
                readers.setdefault(t, []).append(i)
            for t in op.writes:
                if op.grp is not None and last_w.get(t) and ops[last_w[t][0]].grp == op.grp:
                    last_w[t].append(i)
                else:
                    last_w[t] = [i]
                readers[t] = []
            if op.dma:
                dma_since.append(i)
            else:
                last_eng[op.eng] = i
        for op in ops:
            if op.dma:
                op.sig = True
        final_ops = []
        for t in final_wait_tensors:
            for j in last_w.get(t, ()):
                ops[j].sig = True
                final_ops.append(j)
        esem = {}
        ecnt = {}
        for e in (PE, ACT, DVE, POOL):
            esem[e] = nc.alloc_semaphore(name="s_" + e)
            ecnt[e] = 0
        dsem = {}
        dcnt = {}
        waited = {e: {} for e in self.engs}
        nwaits = 0
        for i, op in enumerate(ops):
            if op.bar:
                continue
            engobj = self.engs[op.eng]
            need = {}
            for j in op.deps:
                sm, v = ops[j].semval
                if need.get(sm.name, (None, -1))[1] < v:
                    need[sm.name] = (sm, v)
            for nm, (sm, v) in need.items():
                if waited[op.eng].get(nm, -1) >= v:
                    continue
                engobj.wait_ge(sm, v)
                nwaits += 1
                waited[op.eng][nm] = v
            ins = getattr(engobj, op.fn)(*op.args, **op.kw)
            if op.sig:
                if op.dma:
                    key = sorted(op.writes)[0]
                    if key not in dsem:
                        dsem[key] = nc.alloc_semaphore(name="d_" + key)
                        dcnt[key] = 0
                    dcnt[key] += 16
                    ins.then_inc(dsem[key], 16)
                    op.semval = (dsem[key], dcnt[key])
                else:
                    ecnt[op.eng] += 1
                    ins.then_inc(esem[op.eng], 1)
                    op.semval = (esem[op.eng], ecnt[op.eng])
        for j in final_ops:
            s, v = ops[j].semval
            if waited[SP].get(s.name, -1) >= v:
                continue
            nc.sync.wait_ge(s, v)
            waited[SP][s.name] = v
        print(f"[fw] ops={len(ops)} waits={nwaits} " + " ".join(f"{e}={ecnt[e]}" for e in ecnt) + f" dma_sems={len(dsem)}", flush=True)

D = 2048
HD = 128
EPS = 1e-6
NEG = -30000.0
SCALE = HD ** -0.5
NEXP = 32

CHUNKS = ([("T", 0), ("T", 2), ("N", (2, "K", 0)), ("N", (3, "K", 2)), ("V", 0), ("V", 2)]
          + [("N", (5, "K", 4 + 2 * i)) for i in range(4)] + [("V", 4 + 2 * i) for i in range(4)] + [("F", 0)]
          + [("N", (6, "Q", 2 * i)) for i in range(4)] + [("N", (7, "Q", 8 + 2 * i)) for i in range(4)])
NCH = len(CHUNKS)


def build(stop=99, nexp=NEXP, dbg_names=()):
    nc = bass.Bass("TRN2", target_bir_lowering=False)
    k = K(nc)
    used_inputs = []
    dbg_out = []

    def din(name, shape):
        used_inputs.append(name)
        return nc.dram_tensor(name, list(shape), F32, kind="ExternalInput").ap()

    def dscr(name, shape, dt=BF16):
        return nc.dram_tensor(name, list(shape), dt, kind="Internal").ap()

    class Arena:
        def __init__(self, base, size):
            self.base, self.size, self.off = base, size, 0

        def reset(self):
            self.off = 0

        def __call__(self, name, shape, dt=F32):
            nb = int(np.prod(shape[1:])) * (2 if dt == BF16 else 4)
            nb = (nb + 31) // 32 * 32
            assert self.off + nb <= self.size, (name, self.off, nb, self.size)
            t = nc.alloc_sbuf_tensor_at(name, list(shape), dt, offset=self.base + self.off)
            self.off += nb
            return t.ap()

    KB = 1024
    PA = Arena(16 * KB, 44 * KB)
    WA = Arena(60 * KB, 32 * KB)
    B1 = Arena(92 * KB, 64 * KB)
    X2A = Arena(156 * KB, 64 * KB)
    PS = [nc.alloc_psum_tensor(f"ps{i}", [128, 512], F32).ap() for i in range(8)]
    WB = [WA(f"wb{i}", [128, 16, 256], BF16) for i in range(4)]
    cnt = {"w": 0, "p": 0, "t": 0, "o": 0, "e": 0}

    def nxt(key, n):
        v = cnt[key] % n
        cnt[key] += 1
        return v

    def dbg(name, ap):
        if name in dbg_names:
            shp = list(ap.shape)
            o = nc.dram_tensor("dbg_" + name, shp, ap.dtype if hasattr(ap, "dtype") else F32, kind="ExternalOutput").ap()
            k.dma(o, ap, eng=SP)
            dbg_out.append("dbg_" + name)

    def finish():
        k.emit(final_wait_tensors=["out"] + dbg_out if stop >= 12 else dbg_out)
        return nc, used_inputs, dbg_out

    identF = PA("identF", [128, 128]); k.dma(identF, din("identF", [128, 128]))
    identB = PA("identB", [128, 128], BF16); k.copy(identB, identF)
    cT = PA("cT", [128, 16]); k.dma(cT, din("cT", [128, 16]))
    na = PA("na", [128, 16]); k.dma(na, din("na", [128, 16]))
    nm_ = PA("nm", [128, 16]); k.dma(nm_, din("nm", [128, 16]))
    cols = PA("cols", [128, 6, 16])
    A1 = PA("A1", [128, 16]); A2 = PA("A2", [128, 16])
    gAB = PA("gAB", [128, 2048]); gMB = PA("gMB", [128, 2048])
    gB = {2: gAB, 5: gMB}
    fgsb = PA("fgsb", [128, 16, 32])
    small = PA("small", [128, 64])
    ss, rs, rstd = small[:, 0:1], small[:, 1:2], small[:, 2:3]
    ssq, rq, rstdq = small[:, 4:6], small[:, 6:8], small[:, 8:10]
    rc_, fac_ = small[:, 10:12], small[:, 12:14]

    X2A.reset()
    sc_bf = X2A("sc_bf", [128, 16], BF16)
    scB = X2A("scB", [128, 16, 128], BF16)
    k.act(sc_bf, cT, AF.Silu)
    for kt in range(16):
        k.copy(scB[:, kt, :], sc_bf[:, kt:kt + 1].to_broadcast([128, 128]))
    modw = din("modw", [2048, 12288]).rearrange("(kt p) n -> p kt n", p=128)
    modbB = din("modbB", [128, 12288])
    mbs = [X2A(f"mb{i}", [128, 256]) for i in range(2)]
    tmp2 = X2A("tmp2", [128, 256]); tmp3 = X2A("tmp3", [128, 128])
    for ch in range(48):
        v, sub = ch // 8, ch % 8
        wb = WB[nxt("w", 4)]
        k.dma(wb, modw[:, :, ch * 256:(ch + 1) * 256], eng=POOL)
        mb = mbs[ch % 2]
        k.dma(mb, modbB[:, ch * 256:(ch + 1) * 256])
        ps = PS[nxt("p", 4)][:, 0:256]
        for kt in range(16):
            k.mm(ps, scB[:, kt, :], wb[:, kt, :], start=(kt == 0), stop=(kt == 15))
        if v in gB:
            k.tt(gB[v][:, sub * 256:(sub + 1) * 256], ps, mb, ALU.add)
        else:
            k.tt(tmp2, ps, mb, ALU.add)
            for jj in range(2):
                kt = sub * 2 + jj
                k.tt(tmp3, tmp2[:, jj * 128:(jj + 1) * 128], identF, ALU.mult)
                k.reduce_sum(cols[:, v, kt:kt + 1], tmp3)
    k.ts(A1, cols[:, 1, :], 1.0, None, ALU.add); k.tt(A1, A1, na, ALU.mult)
    k.ts(A2, cols[:, 4, :], 1.0, None, ALU.add); k.tt(A2, A2, nm_, ALU.mult)
    Bs1, Bs2 = cols[:, 0, :], cols[:, 3, :]
    dbg("A1", A1); dbg("gAB", gAB)
    if stop <= 1:
        return finish()

    k.barrier()
    X2A.reset(); B1.reset()
    hT = B1("hT", [128, 16, 2048], BF16)
    xts = [X2A(f"xt{i}", [128, 2048]) for i in range(2)]
    junk = X2A("junk", [128, 2048], BF16)
    xn = X2A("xn", [128, 2048], BF16)
    xloc = din("xloc", [2048, 2048])

    def norm_transpose(src, dstT, col0, Acol, Bcol):
        k.act(junk, src, AF.Square, accum_out=ss)
        k.act(rs, ss, AF.Sqrt, bias=EPS, scale=1.0 / D)
        k.recip(rstd, rs)
        k.ts(xn, src, rstd, None, ALU.mult)
        for k4 in range(4):
            pt = PS[4 + nxt("t", 2)]
            for j in range(4):
                kt = k4 * 4 + j
                k.mm(pt[:, j * 128:(j + 1) * 128], xn[:, kt * 128:(kt + 1) * 128], identB)
            for j in range(4):
                kt = k4 * 4 + j
                if j % 2 == 0:
                    k.act(dstT[:, kt, col0:col0 + 128], pt[:, j * 128:(j + 1) * 128], AF.Identity, bias=Bcol[:, kt:kt + 1], scale=Acol[:, kt:kt + 1])
                else:
                    k.ts(dstT[:, kt, col0:col0 + 128], pt[:, j * 128:(j + 1) * 128], Acol[:, kt:kt + 1], Bcol[:, kt:kt + 1], ALU.mult, ALU.add)

    for m in range(16):
        xt = xts[m % 2]
        k.dma(xt, xloc[m * 128:(m + 1) * 128, :])
        norm_transpose(xt, hT, m * 128, A1, Bs1)
    dbg("hT", hT[:, 0, :])
    if stop <= 2:
        return finish()

    k.barrier()
    X2A.reset()
    KTs = dscr("KTs", [12, 128, 2048]); KCr = dscr("KCr", [4, 128, 2048])
    Vs = dscr("Vs", [2048, 12, 128]); QTs = dscr("QTs", [16, 128, 1024])
    gt = X2A("gt", [128, 8, 128])
    gt_in = din("gt", [128, 6, 128])
    k.dma(gt[:, 0:6, :], gt_in)
    k.ts(gt[:, 6, :], gt[:, 0, :], SCALE, None, ALU.mult)
    k.ts(gt[:, 7, :], gt[:, 4, :], SCALE, None, ALU.mult)
    stgs = [X2A(f"stg{i}", [128, 256], BF16) for i in range(4)]
    st2s = [X2A(f"st2{i}", [128, 2, 128], BF16) for i in range(4)]
    junkh = X2A("junkh", [128, 128], BF16)
    winp = din("winp", [2048, NCH * 256]).rearrange("(kt p) n -> p kt n", p=128)
    for ci, (kind, arg) in enumerate(CHUNKS):
        wb = WB[nxt("w", 4)]
        k.dma(wb, winp[:, :, ci * 256:(ci + 1) * 256], eng=POOL)
        tiles = range(16) if ci < 15 else range(8, 16)
        for m in tiles:
            ps = PS[nxt("p", 4)][:, 0:256]
            for kt in range(16):
                k.mm(ps, hT[:, kt, m * 128:(m + 1) * 128], wb[:, kt, :], start=(kt == 0), stop=(kt == 15))
            if kind == "F":
                k.copy(fgsb[:, m, :], ps[:, 0:32], eng=ACT)
                continue
            stg = stgs[nxt("e", 4)]
            if kind == "V":
                k.copy(stg, ps, eng=ACT)
                k.dma(Vs[m * 128:(m + 1) * 128, arg:arg + 2, :], stg.rearrange("p (j d) -> p j d", j=2), grp="s3")
                continue
            if kind == "T":
                k.copy(stg, ps, eng=ACT)
            else:
                gi = arg[0]
                for j in range(2):
                    k.act(junkh, ps[:, j * 128:(j + 1) * 128], AF.Square, accum_out=ssq[:, j:j + 1])
                k.act(rq, ssq, AF.Sqrt, bias=EPS, scale=1.0 / HD)
                k.recip(rstdq, rq)
                for j in range(2):
                    k.stt(stg[:, j * 128:(j + 1) * 128], ps[:, j * 128:(j + 1) * 128], rstdq[:, j:j + 1], gt[:, gi, :], ALU.mult, ALU.mult)
            pt = PS[4 + nxt("t", 2)]
            for j in range(2):
                k.mm(pt[:, j * 128:(j + 1) * 128], stg[:, j * 128:(j + 1) * 128], identB)
            st2 = st2s[nxt("o", 4)]
            k.copy(st2, pt[:, 0:256].rearrange("p (j t) -> p j t", j=2))
            if kind == "T":
                dst = KCr[arg:arg + 2, :, m * 128:(m + 1) * 128]
            elif arg[1] == "K":
                dst = KTs[arg[2]:arg[2] + 2, :, m * 128:(m + 1) * 128]
            else:
                dst = QTs[arg[2]:arg[2] + 2, :, (m - 8) * 128:(m - 7) * 128]
            k.dma(dst.rearrange("j d t -> d j t"), st2, grp="s3")
    if "QTs" in dbg_names:
        qd = X2A("qd", [128, 1024], BF16); k.dma(qd, QTs[0]); dbg("QTs", qd)
        kd = X2A("kd", [128, 2048], BF16); k.dma(kd, KTs[4]); dbg("KTs", kd)
    dbg("fgsb", fgsb)
    if stop <= 3:
        return finish()

    k.barrier()
    X2A.reset(); B1.reset()
    def cload(ar, name, shape, dt=F32, eng=SP, src=None):
        t = ar(name, shape, dt)
        k.dma(t, src if src is not None else din(name, shape), eng=(POOL if dt == BF16 else eng))
        return t
    triF = cload(X2A, "triF", [128, 128]); onesF = cload(X2A, "onesF", [128, 128]); e64 = cload(X2A, "e64", [128, 128])
    fbias = cload(X2A, "fbias", [128, 8]); gbias = cload(X2A, "gbias", [128, 24])
    padk8 = cload(PA, "padk8", [128, 16, 8]); farb = cload(X2A, "farb", [128, 8])
    padn = cload(PA, "padn", [128, 1])
    padfar = PA("padfar", [128, 16, 8]); biasF = PA("biasF", [128, 8, 16, 8]); gsig = PA("gsig", [128, 8, 24])
    fl = X2A("fl", [128, 16, 8]); sg_ = X2A("sg_", [128, 128]); ls = X2A("ls", [128, 128])
    wsb = X2A("wsb", [128, 128]); tot = X2A("tot", [128, 16, 8]); offs = X2A("offs", [128, 16, 8])
    cum = X2A("cum", [128, 16, 8]); crefs = X2A("crefs", [128, 16, 8]); cumpad = X2A("cumpad", [128, 16, 8])
    for m in range(16):
        k.tt(fl[:, m, :], fgsb[:, m, 0:8], fbias, ALU.add)
        k.tt(padfar[:, m, :], padk8[:, m, :], farb, ALU.add)
    flf = fl.rearrange("p m h -> p (m h)")
    k.act(sg_, flf, AF.Sigmoid); k.act(ls, sg_, AF.Ln)
    k.mm(PS[0][:, 0:128], triF, ls); k.mm(PS[1][:, 0:128], onesF, ls)
    k.copy(wsb, PS[0][:, 0:128]); k.copy(tot.rearrange("p m h -> p (m h)"), PS[1][:, 0:128])
    k.memset(offs[:, 0, :], 0.0)
    for m in range(1, 16):
        k.tt(offs[:, m, :], offs[:, m - 1, :], tot[:, m - 1, :], ALU.add)
    cumf = cum.rearrange("p m h -> p (m h)")
    k.tt(cumf, wsb, offs.rearrange("p m h -> p (m h)"), ALU.add)
    k.mm(PS[2][:, 0:128], e64, cumf)
    k.copy(crefs.rearrange("p m h -> p (m h)"), PS[2][:, 0:128])
    k.tt(cumpad, cum, padk8, ALU.subtract)
    for i in range(8):
        for kb in range(9 + i):
            k.tt(biasF[:, i, kb, :], crefs[:, 8 + i, :], cumpad[:, kb, :], ALU.subtract)
        k.tt(gsig[:, i, :], fgsb[:, 8 + i, 8:32], gbias, ALU.add)
    k.act(gsig.rearrange("p i g -> p (i g)"), gsig.rearrange("p i g -> p (i g)"), AF.Sigmoid)
    dbg("biasF", biasF.rearrange("p i k h -> p (i k h)")); dbg("gsig", gsig.rearrange("p i g -> p (i g)"))
    if stop <= 4:
        return finish()

    k.barrier()
    X2A.reset()
    oacc = X2A("oacc", [128, 8, 1024])
    B1.reset()
    AT = Arena(B1.base, 32 * KB)
    mixT = Arena(B1.base + 32 * KB, 32 * KB)("mixT", [128, 16, 1024], BF16)
    XT = Arena(X2A.base + 32 * KB, 32 * KB)
    gtc = XT("gtc", [128, 6, 128]); k.dma(gtc, gt_in)
    KCT = [PA(f"KCT{g}", [128, 127], BF16) for g in range(2)]
    VCa = [PA(f"VCa{g}", [128, 161], BF16) for g in range(2)]
    ovl_in = din("ovl", [127, 32])
    for g in range(2):
        k.memset(VCa[g][:, 128:129], 1.0)
        k.dma(VCa[g][0:127, 129:161], ovl_in, eng=POOL)
    posT = cload(XT, "posT", [128, 2, 32])
    w1_in = din("w1", [2, 4096, 256]); w2_in = din("w2", [2, 256, 128])
    w1sb = AT("w1sb", [128, 32, 256], BF16); w2sb = AT("w2sb", [128, 2, 128], BF16)
    kcraw = AT("kcraw", [128, 2048], BF16); tmpl = AT("tmpl", [128, 32, 127], BF16)
    GT = [AT(f"GT{i}", [128, 127], BF16) for i in range(2)]
    gx2 = XT("gx2", [128, 127]); gu = XT("gu", [128, 127]); gth = XT("gth", [128, 127])
    kcn = XT("kcn", [128, 128], BF16); junkc = XT("junkc", [128, 128], BF16)
    for kv in range(2):
        k.dma(w1sb, w1_in[kv].rearrange("(l d) h -> d l h", d=128), eng=POOL)
        k.dma(w2sb, w2_in[kv].rearrange("(hh p) d -> p hh d", p=128), eng=POOL)
        for g in range(2):
            k.dma(kcraw, KCr[kv * 2 + g])
            for l in range(32):
                k.ts(tmpl[:, l, :], kcraw[:, l:l + 2017:16], posT[:, kv, l:l + 1], None, ALU.add, eng=(DVE if l % 2 == 0 else POOL))
            for hh in range(2):
                ps = PS[hh][:, 0:127]
                for l in range(32):
                    k.mm(ps, w1sb[:, l, hh * 128:(hh + 1) * 128], tmpl[:, l, :], start=(l == 0), stop=(l == 31))
                k.act(gx2, ps, AF.Square)
                k.ts(gu, gx2, 0.044715, 1.0, ALU.mult, ALU.add)
                k.tt(gu, gu, ps, ALU.mult)
                k.act(gth, gu, AF.Tanh, scale=0.7978845608028654)
                k.ts(gth, gth, 0.5, 0.5, ALU.mult, ALU.add)
                k.tt(GT[hh], gth, ps, ALU.mult)
            pk = PS[2][0:127, 0:128]
            k.mm(pk, GT[0], w2sb[:, 0, :], start=True, stop=False)
            k.mm(pk, GT[1], w2sb[:, 1, :], start=False, stop=True)
            if kv == 0:
                k.act(junkc[0:127, :], pk, AF.Square, accum_out=ss[0:127, :])
                k.act(rs[0:127, :], ss[0:127, :], AF.Sqrt, bias=EPS, scale=1.0 / HD)
                k.recip(rstd[0:127, :], rs[0:127, :])
                k.stt(kcn[0:127, :], pk, rstd[0:127, :], gtc[0:127, 1, :], ALU.mult, ALU.mult)
                k.mm(PS[4][:, 0:127], kcn[0:127, :], identB[0:127, 0:127])
                k.copy(KCT[g], PS[4][:, 0:127])
            else:
                k.copy(VCa[g][0:127, 0:128], pk)
    dbg("KCT0", KCT[0]); dbg("VCa0", VCa[0])
    if stop <= 5:
        return finish()

    k.barrier()
    AT.reset(); XT.reset()
    qTs = [AT(f"qT{i}", [128, 1024], BF16) for i in range(2)]
    bcs = [AT(f"bc{i}", [128, 1024], BF16) for i in range(2)]
    pTs = [AT(f"pT{i}", [128, 512], BF16) for i in range(4)]
    vAs = [AT(f"vA{i}", [128, 16, 129], BF16) for i in range(2)]
    for v_ in vAs:
        k.memset(v_[:, :, 128:129], 1.0)
    negselT = [AT(f"negsel{g}", [32, 1024], BF16) for g in range(2)]
    t01 = cload(AT, "t01", [128, 8, 2, 128], BF16)
    wm4 = cload(AT, "wm4", [128, 128], BF16); causalB = cload(AT, "causal", [128, 128], BF16)
    imp = XT("imp", [128, 8, 32]); vmul = cload(XT, "vmul", [128, 8, 32]); amask = cload(XT, "amask", [128, 8, 32])
    impm = XT("impm", [128, 32]); wk_ = XT("wk_", [128, 32]); m8 = XT("m8", [128, 16]); nsel = XT("nsel", [128, 32], BF16)
    onormB = cload(XT, "onormB", [128, 2048])
    kTs = [XT(f"kT{i}", [128, 2048], BF16) for i in range(2)]
    ekb = cload(XT, "ekb", [32, 16, 128], BF16)
    mix = XT("mix", [128, 1024], BF16); junko = XT("junko", [128, 1024], BF16)
    biascmp_in = din("biascmp", [8, 127, 1024])
    qcnt = [0]

    def load_q(hq):
        q = qTs[qcnt[0] % 2]; qcnt[0] += 1
        k.dma(q, QTs[hq])
        return q

    for g in range(2):
        for r in range(4):
            h = 4 * g + r
            qT = load_q(h)
            bc = bcs[h % 2]
            k.dma(bc[0:127, :], biascmp_in[h], eng=POOL)
            for half in range(2):
                ps = PS[nxt("p", 4)][0:127, :]
                k.mm(ps, KCT[g], qT[:, half * 512:(half + 1) * 512], start=True, stop=False)
                k.mm(ps, identB[0:127, 0:127], bc[0:127, half * 512:(half + 1) * 512], start=False, stop=True)
                pT = pTs[nxt("e", 4)]
                k.act(pT[0:127, :], ps, AF.Exp, bias=padn[0:127, :])
                for i4 in range(4):
                    i = half * 4 + i4
                    po = PS[6 + nxt("o", 2)][:, 0:161]
                    k.mm(po, pT[0:127, i4 * 128:(i4 + 1) * 128], VCa[g][0:127, :])
                    k.ts(rc_[:, 1:2], po[:, 128:129], 1e-30, None, ALU.max)
                    k.recip(rc_[:, 0:1], rc_[:, 1:2])
                    k.tt(fac_[:, 0:1], rc_[:, 0:1], gsig[:, i, h:h + 1], ALU.mult)
                    k.ts(oacc[:, i, h * 128:(h + 1) * 128], po[:, 0:128], fac_[:, 0:1], None, ALU.mult)
                    if r == 0:
                        k.ts(imp[:, i, :], po[:, 129:161], rc_[:, 0:1], None, ALU.mult)
                    else:
                        k.stt(imp[:, i, :], po[:, 129:161], rc_[:, 0:1], imp[:, i, :], ALU.mult, ALU.add)
        for i in range(8):
            k.tt(impm, imp[:, i, :], vmul[:, i, :], ALU.mult)
            k.tt(impm, impm, amask[:, i, :], ALU.add)
            k.max8(m8[:, 0:8], impm)
            k.match_replace(wk_, m8[:, 0:8], impm, -1e9)
            k.max8(m8[:, 8:16], wk_)
            k.ts(nsel, impm, m8[:, 15:16], -NEG, ALU.is_ge, ALU.mult)
            k.ts(nsel, nsel, NEG, None, ALU.add)
            k.mm(PS[4][0:32, 0:128], nsel, identB)
            k.copy(negselT[g][:, i * 128:(i + 1) * 128], PS[4][0:32, 0:128])
    dbg("oacc_cmp", oacc[:, 0, :]); dbg("negsel0", negselT[0])
    if stop <= 6:
        return finish()

    kvc = [0]

    def load_kv(idx):
        kT = kTs[kvc[0] % 2]; vA = vAs[kvc[0] % 2]; kvc[0] += 1
        k.dma(kT, KTs[idx])
        k.dma(vA[:, :, 0:128], Vs[:, idx, :].rearrange("(m p) d -> p m d", p=128))
        return kT, vA

    def attend(kT, vA, qT, h, i, kbs, mode, g):
        qb = 8 + i
        po = PS[6 + nxt("o", 2)][:, 0:129]
        for kb in kbs:
            dl = qb - kb
            ps = PS[nxt("p", 4)][:, 0:128]
            extra = []
            if mode == "fox":
                if dl == 0:
                    extra.append((identB, causalB))
                bias = biasF[:, i, kb, h:h + 1]
            else:
                if dl <= 1:
                    extra.append((identB, t01[:, h, dl, :]))
                    bias = padk8[:, kb, 0:1]
                else:
                    bias = padfar[:, kb, h:h + 1]
                    if mode == "win" and dl == 4:
                        extra.append((identB, wm4))
                if mode == "slc":
                    extra.append((ekb[:, kb, :], negselT[g][:, i * 128:(i + 1) * 128]))
            k.mm(ps, kT[:, kb * 128:(kb + 1) * 128], qT[:, i * 128:(i + 1) * 128], start=True, stop=(len(extra) == 0))
            for xi, (l_, r_) in enumerate(extra):
                k.mm(ps, l_, r_, start=False, stop=(xi == len(extra) - 1))
            pT = pTs[nxt("e", 4)][:, 0:128]
            k.act(pT, ps, AF.Exp, bias=bias)
            k.mm(po, pT, vA[:, kb, :], start=(kb == kbs[0]), stop=(kb == kbs[-1]))
        return po

    for g in range(2):
        for mode, kidx, goff in (("slc", g, 8), ("win", 2 + g, 16)):
            kT, vA = load_kv(kidx)
            for r in range(4):
                h = 4 * g + r
                qT = load_q(h)
                for i in range(8):
                    qb = 8 + i
                    kbs = list(range(qb + 1)) if mode == "slc" else list(range(qb - 4, qb + 1))
                    po = attend(kT, vA, qT, h, i, kbs, mode, g)
                    k.recip(rc_[:, 0:1], po[:, 128:129])
                    k.tt(fac_[:, 0:1], rc_[:, 0:1], gsig[:, i, goff + h:goff + h + 1], ALU.mult)
                    k.stt(oacc[:, i, h * 128:(h + 1) * 128], po[:, 0:128], fac_[:, 0:1], oacc[:, i, h * 128:(h + 1) * 128], ALU.mult, ALU.add)

    def out_norm(koff):
        for i in range(8):
            k.act(junko, oacc[:, i, :], AF.Square, accum_out=ss)
            k.act(rs, ss, AF.Sqrt, bias=EPS, scale=1.0 / 1024)
            k.recip(rstd, rs)
            k.stt(mix, oacc[:, i, :], rstd, onormB[:, koff * 128:koff * 128 + 1024], ALU.mult, ALU.mult)
            for k4 in range(2):
                pt = PS[4 + nxt("t", 2)]
                for j in range(4):
                    kt = k4 * 4 + j
                    k.mm(pt[:, j * 128:(j + 1) * 128], mix[:, kt * 128:(kt + 1) * 128], identB)
                k.copy(mixT[:, koff + k4 * 4:koff + k4 * 4 + 4, i * 128:(i + 1) * 128], pt.rearrange("p (j t) -> p j t", j=4), eng=(ACT if k4 == 0 else DVE))

    dbg("oacc_nsa", oacc[:, 0, :])
    out_norm(0)
    if stop <= 7:
        dbg("mixT", mixT[:, 0, :])
        return finish()
    for h in range(8):
        kT, vA = load_kv(4 + h)
        qT = load_q(8 + h)
        for i in range(8):
            po = attend(kT, vA, qT, h, i, list(range(9 + i)), "fox", 0)
            k.recip(rc_[:, 0:1], po[:, 128:129])
            k.ts(oacc[:, i, h * 128:(h + 1) * 128], po[:, 0:128], rc_[:, 0:1], None, ALU.mult)
    dbg("oacc_fox", oacc[:, 0, :])
    out_norm(8)
    dbg("mixT", mixT[:, 8, :])
    if stop <= 8:
        return finish()

    k.barrier()
    X2A.reset(); AT.reset()
    x2 = X2A("x2", [128, 8, 2048])
    for i in range(8):
        k.dma(x2[:, i, :], xloc[(8 + i) * 128:(9 + i) * 128, :])
    tmps = [AT(f"tmpo{i}", [128, 512]) for i in range(3)]
    wout = din("wout", [2048, 2048]).rearrange("(kt p) n -> p kt n", p=128)
    for c in range(8):
        wb = WB[nxt("w", 4)]
        k.dma(wb, wout[:, :, c * 256:(c + 1) * 256], eng=POOL)
        for i in range(8):
            ps = PS[nxt("p", 4)][:, 0:256]
            for kt in range(16):
                k.mm(ps, mixT[:, kt, i * 128:(i + 1) * 128], wb[:, kt, :], start=(kt == 0), stop=(kt == 15))
            tmp = tmps[nxt("e", 3)][:, 0:256]
            k.tt(tmp, ps, gAB[:, c * 256:(c + 1) * 256], ALU.mult)
            k.tt(x2[:, i, c * 256:(c + 1) * 256], x2[:, i, c * 256:(c + 1) * 256], tmp, ALU.add, eng=POOL)
    dbg("x2", x2[:, 0, :])
    if stop <= 9:
        return finish()

    k.barrier()
    B1.reset()
    h2T = B1("h2T", [128, 16, 1024], BF16)
    actT = B1("actT", [128, 16, 1024], BF16)
    MT = Arena(B1.base + 32 * KB, 32 * KB)
    junk2 = MT("junk2", [128, 2048], BF16); xn2 = MT("xn2", [128, 2048], BF16)
    junk, xn = junk2, xn2
    def norm_transpose2(src, dstT, col0, Acol, Bcol):
        k.act(junk2, src, AF.Square, accum_out=ss)
        k.act(rs, ss, AF.Sqrt, bias=EPS, scale=1.0 / D)
        k.recip(rstd, rs)
        k.ts(xn2, src, rstd, None, ALU.mult)
        for k4 in range(4):
            pt = PS[4 + nxt("t", 2)]
            for j in range(4):
                kt = k4 * 4 + j
                k.mm(pt[:, j * 128:(j + 1) * 128], xn2[:, kt * 128:(kt + 1) * 128], identB)
            for j in range(4):
                kt = k4 * 4 + j
                if j % 2 == 0:
                    k.act(dstT[:, kt, col0:col0 + 128], pt[:, j * 128:(j + 1) * 128], AF.Identity, bias=Bcol[:, kt:kt + 1], scale=Acol[:, kt:kt + 1])
                else:
                    k.ts(dstT[:, kt, col0:col0 + 128], pt[:, j * 128:(j + 1) * 128], Acol[:, kt:kt + 1], Bcol[:, kt:kt + 1], ALU.mult, ALU.add)
    for i in range(8):
        norm_transpose2(x2[:, i, :], h2T, i * 128, A2, Bs2)
    rwsb = MT("rwsb", [128, 16, 32], BF16)
    k.dma(rwsb, din("rw", [2048, 32]).rearrange("(kt p) e -> p kt e", p=128), eng=POOL)
    rbB = cload(MT, "rbB", [128, 32])
    gates = PA("gates", [128, 8, 32]); bupT = cload(PA, "bupT", [128, 32, 32])
    lg = MT("lg", [128, 32]); m8r = MT("m8r", [128, 8]); nmx = MT("nmx", [128, 1]); msk = MT("msk", [128, 32])
    ex = MT("ex", [128, 32]); rsm = MT("rsm", [128, 1]); rrm = MT("rrm", [128, 1])
    gTs = MT("gTs", [32, 128]); bdn = cload(MT, "bdn", [32, 2048])
    tmpb = [MT(f"tmpb{i}", [128, 512]) for i in range(2)]
    for i in range(8):
        ps = PS[nxt("p", 4)][:, 0:32]
        for kt in range(16):
            k.mm(ps, h2T[:, kt, i * 128:(i + 1) * 128], rwsb[:, kt, :], start=(kt == 0), stop=(kt == 15))
        k.tt(lg, ps, rbB, ALU.add)
        k.max8(m8r, lg)
        k.ts(nmx, m8r[:, 0:1], -1.0, None, ALU.mult)
        k.ts(msk, lg, m8r[:, 3:4], None, ALU.is_ge)
        k.act(ex, lg, AF.Exp, bias=nmx)
        k.tt(ex, ex, msk, ALU.mult)
        k.reduce_sum(rsm, ex)
        k.recip(rrm, rsm)
        k.ts(gates[:, i, :], ex, rrm, None, ALU.mult)
        k.mm(PS[4][0:32, 0:128], gates[:, i, :], identF)
        k.copy(gTs, PS[4][0:32, 0:128])
        for c in range(4):
            pb = PS[nxt("p", 4)]
            k.mm(pb, gTs, bdn[:, c * 512:(c + 1) * 512])
            tb = tmpb[c % 2]
            k.tt(tb, pb, gMB[:, c * 512:(c + 1) * 512], ALU.mult)
            k.tt(x2[:, i, c * 512:(c + 1) * 512], x2[:, i, c * 512:(c + 1) * 512], tb, ALU.add, eng=POOL)
    dbg("gates", gates.rearrange("p i e -> p (i e)")); dbg("h2T", h2T[:, 0, :])
    if stop <= 10:
        return finish()

    k.barrier()
    ET = Arena(PA.base + PA.off, PA.size - PA.off)
    tg = ET("tg", [128, 512]); tsg = ET("tsg", [128, 512]); tl = ET("tl", [128, 512]); ta = ET("ta", [128, 512])
    tds = [ET(f"td{i}", [128, 256]) for i in range(2)]
    wup = din("wup", [NEXP, 2048, 4096]); wdn = din("wdn", [NEXP, 2048, 2048])
    pset = [0]
    for e in range(nexp):
        wu = wup[e].rearrange("(kt p) f -> p kt f", p=128)
        for c in range(8):
            wg = WB[nxt("w", 4)]; k.dma(wg, wu[:, :, c * 256:(c + 1) * 256], eng=POOL)
            wl = WB[nxt("w", 4)]; k.dma(wl, wu[:, :, 2048 + c * 256:2048 + (c + 1) * 256], eng=POOL)
            for j in range(2):
                ft = c * 2 + j
                base = 4 * (pset[0] % 2); pset[0] += 1
                for kt in range(16):
                    for tc in range(2):
                        k.mm(PS[base + tc], wg[:, kt, j * 128:(j + 1) * 128], h2T[:, kt, tc * 512:(tc + 1) * 512], start=(kt == 0), stop=(kt == 15))
                for kt in range(16):
                    for tc in range(2):
                        k.mm(PS[base + 2 + tc], wl[:, kt, j * 128:(j + 1) * 128], h2T[:, kt, tc * 512:(tc + 1) * 512], start=(kt == 0), stop=(kt == 15))
                for tc in range(2):
                    k.ts(tg, PS[base + tc], bupT[:, e, ft:ft + 1], 7.0, ALU.add, ALU.min)
                    k.act(tsg, tg, AF.Sigmoid, scale=1.702)
                    k.ts(tl, PS[base + 2 + tc], bupT[:, e, 16 + ft:17 + ft], 7.0, ALU.add, ALU.min)
                    k.ts(tl, tl, -7.0, 1.0, ALU.max, ALU.add)
                    k.tt(ta, tg, tsg, ALU.mult, eng=POOL)
                    k.tt(actT[:, ft, tc * 512:(tc + 1) * 512], ta, tl, ALU.mult, eng=POOL)
        wd_ = wdn[e].rearrange("(kt p) d -> p kt d", p=128)
        for c in range(8):
            wd = WB[nxt("w", 4)]; k.dma(wd, wd_[:, :, c * 256:(c + 1) * 256], eng=POOL)
            for i in range(8):
                ps = PS[nxt("p", 8)][:, 0:256]
                for kt in range(16):
                    k.mm(ps, actT[:, kt, i * 128:(i + 1) * 128], wd[:, kt, :], start=(kt == 0), stop=(kt == 15))
                td = tds[nxt("e", 2)]
                k.tt(td, ps, gMB[:, c * 256:(c + 1) * 256], ALU.mult)
                k.stt(x2[:, i, c * 256:(c + 1) * 256], td, gates[:, i, e:e + 1], x2[:, i, c * 256:(c + 1) * 256], ALU.mult, ALU.add)

    outd = nc.dram_tensor("out", [1024, 2048], F32, kind="ExternalOutput").ap()
    for i in range(8):
        k.dma(outd[i * 128:(i + 1) * 128, :], x2[:, i, :])
    stop = 12
    return finish()


def _t5_bucket(dist):
    n = np.maximum(dist, 0)
    nf = np.maximum(n, 1).astype(np.float32)
    large = 16 + (np.log(nf / np.float32(16)) / np.float32(math.log(8.0)) * np.float32(16)).astype(np.int32)
    large = np.minimum(large, 31)
    return np.where(n < 16, n, large)


def host_prep(inp, core):
    f32 = np.float32
    b, p = core // 2, core % 2
    x = np.asarray(inp["x"], f32)
    o = {}
    if p == 1:
        o["xloc"] = np.ascontiguousarray(x[b])
    else:
        o["xloc"] = np.concatenate([np.zeros((1024, D), f32), x[b, :1024]], axis=0)
    col = lambda v: np.ascontiguousarray(np.asarray(v, f32).reshape(16, 128).T)
    rep = lambda v: np.ascontiguousarray(np.broadcast_to(np.asarray(v, f32).reshape(1, -1), (128, np.asarray(v).size)))
    o["cT"] = col(inp["c"][b])
    o["na"] = col(inp["norm_attn"][0]); o["nm"] = col(inp["norm_moe"][0])
    o["modw"] = np.asarray(inp["mod_w"][0], f32)
    o["modbB"] = rep(inp["mod_b"][0])
    o["identF"] = np.eye(128, dtype=f32)
    w = np.asarray(inp["w_in"][0], f32)
    sizes = [1024, 256, 256, 256, 256, 256, 256, 24, 1024, 1024, 1024, 8]
    offs = np.concatenate([[0], np.cumsum(sizes)])
    seg = lambda i: w[:, offs[i]:offs[i + 1]]
    fcols = np.zeros((D, 256), f32)
    fcols[:, 0:8] = seg(11); fcols[:, 8:32] = seg(7)
    o["winp"] = np.ascontiguousarray(np.concatenate(
        [seg(1), seg(2), seg(3), seg(5), seg(4), seg(6), seg(9), seg(10), fcols, seg(0), seg(8)], axis=1))
    kn = np.asarray(inp["nsa_k_norm"][0], f32)
    gts = [inp["nsa_q_norm"][0], kn[0], kn[1], kn[2], inp["fox_q_norm"][0], inp["fox_k_norm"][0]]
    o["gt"] = np.ascontiguousarray(np.stack([rep(g) for g in gts], axis=1))
    o["fbias"] = rep(inp["fox_forget_bias"][0]); o["gbias"] = rep(inp["b_nsa_gate"][0])
    o["posT"] = np.ascontiguousarray(np.stack([np.asarray(inp["cmp_pos_k"][0], f32).T, np.asarray(inp["cmp_pos_v"][0], f32).T], axis=1))
    o["w1"] = np.ascontiguousarray(np.stack([inp["cmp_k_w1"][0], inp["cmp_v_w1"][0]]).astype(f32))
    o["w2"] = np.ascontiguousarray(np.stack([inp["cmp_k_w2"][0], inp["cmp_v_w2"][0]]).astype(f32))
    n = np.arange(127)[:, None]; j = np.arange(32)[None, :]
    o["ovl"] = ((16 * n < 64 * j + 64) & (16 * n + 32 > 64 * j)).astype(f32)
    rb = np.asarray(inp["rel_bias"], f32)
    t = np.arange(1024)[None, :]
    d = (1024 + t) - (16 * n + 31)
    bc = rb[_t5_bucket(d)]
    bc = np.where((d >= 0)[..., None], bc, f32(NEG))
    o["biascmp"] = np.ascontiguousarray(bc.transpose(2, 0, 1).astype(f32))
    s = np.arange(128)[:, None]; tt_ = np.arange(128)[None, :]
    t01 = np.zeros((128, 8, 2, 128), f32)
    for dl in range(2):
        dd = dl * 128 + tt_ - s
        v = rb[_t5_bucket(dd)]
        v = np.where((dd >= 0)[..., None], v, f32(NEG))
        t01[:, :, dl, :] = v.transpose(0, 2, 1)
    o["t01"] = t01
    o["farb"] = rep(rb[31])
    o["wm4"] = np.where(s > tt_, f32(0), f32(NEG)).astype(f32)
    o["causal"] = np.where(s <= tt_, f32(0), f32(NEG)).astype(f32)
    sl = np.arange(128)[:, None] + 128 * np.arange(16)[None, :]
    padk = np.where((sl < 1024) & (p == 0), f32(NEG), f32(0)).astype(f32)
    o["padk8"] = np.ascontiguousarray(np.broadcast_to(padk[:, :, None], (128, 16, 8)))
    pn = np.zeros((128, 1), f32)
    if p == 0:
        pn[:64] = NEG
    o["padn"] = pn
    tl = (1024 + np.arange(1024)).reshape(8, 128).T
    treal = tl - 1024 * (1 - p)
    cur = (treal // 64)[:, :, None]
    jr = (np.arange(32) - 16 * (1 - p))[None, None, :]
    valid = (jr >= 0) & (jr <= cur)
    f0 = valid & (jr == 0); f1 = valid & (jr == cur) & ~f0; f2 = valid & (jr == cur - 1) & ~f0 & ~f1
    am = np.where(f0, 3e6, np.where(f1, 2e6, np.where(f2, 1e6, np.where(valid, 0.0, -1e6))))
    o["amask"] = am.astype(f32)
    o["vmul"] = (valid & ~f0 & ~f1 & ~f2).astype(f32)
    ek = np.zeros((32, 16, 128), f32)
    for kb in range(16):
        ek[2 * kb, kb, :64] = 1; ek[2 * kb + 1, kb, 64:] = 1
    o["ekb"] = ek
    o["triF"] = (np.arange(128)[:, None] <= np.arange(128)[None, :]).astype(f32)
    o["onesF"] = np.ones((128, 128), f32)
    e64 = np.zeros((128, 128), f32); e64[64, :] = 1
    o["e64"] = e64
    o["onormB"] = rep(inp["out_norm"][0])
    o["wout"] = np.asarray(inp["w_out"][0], f32)
    o["rw"] = np.asarray(inp["router_w"][0], f32)
    o["rbB"] = rep(inp["router_b"][0])
    o["wup"] = np.asarray(inp["exp_w_up"][0], f32)
    o["wdn"] = np.asarray(inp["exp_w_down"][0], f32)
    bu = np.asarray(inp["exp_b_up"][0], f32)
    o["bupT"] = np.ascontiguousarray(bu.reshape(32, 32, 128).transpose(2, 0, 1))
    o["bdn"] = np.asarray(inp["exp_b_down"][0], f32)
    return o


_CACHE = {}


def kernel(**inputs):
    if "prog" not in _CACHE:
        _CACHE["prog"] = build()
    nc, used, _ = _CACHE["prog"]
    in_maps = []
    for core in range(8):
        hp = host_prep(inputs, core)
        in_maps.append({n: hp[n] for n in used})
    res = run_bass_kernel_spmd(nc, in_maps, core_ids=list(range(8)))
    out = np.zeros((4, 2048, D), np.float32)
    for core in range(8):
        b, p = core // 2, core % 2
        out[b, p * 1024:(p + 1) * 1024] = res.results[core]["out"]
    return out
```

```python
import math
import numpy as np
import concourse.bass as bass
import concourse.mybir as mybir
from concourse.bass_utils import run_bass_kernel_spmd

F32 = mybir.dt.float32
BF16 = mybir.dt.bfloat16
ALU = mybir.AluOpType
AF = mybir.ActivationFunctionType
AX = mybir.AxisListType
PE, ACT, DVE, POOL, SP = "pe", "act", "dve", "pool", "sp"


def _tname(ap):
    t = getattr(ap, "tensor", None)
    if t is None:
        t = ap
    return t.name


class Op:
    __slots__ = ("eng", "fn", "args", "kw", "reads", "writes", "dma", "deps", "sig", "semval", "grp", "bar")

    def __init__(self, eng, fn, args, kw, reads, writes, dma=False, grp=None):
        self.eng = eng
        self.fn = fn
        self.args = args
        self.kw = kw
        self.reads = reads
        self.writes = writes
        self.dma = dma
        self.deps = []
        self.sig = False
        self.semval = None
        self.grp = grp
        self.bar = False


class K:
    def __init__(self, nc, same_engine_sync=True):
        self.nc = nc
        self.ops = []
        self.same_engine_sync = same_engine_sync
        self.engs = {PE: nc.tensor, ACT: nc.scalar, DVE: nc.vector, POOL: nc.gpsimd, SP: nc.sync}

    def _rec(self, eng, fn, args, kw, reads, writes, dma=False, grp=None):
        r = set(_tname(a) for a in reads if a is not None and not isinstance(a, (int, float)))
        w = set(_tname(a) for a in writes)
        self.ops.append(Op(eng, fn, args, kw, r, w, dma, grp))

    def barrier(self):
        o = Op(None, None, None, None, set(), set())
        o.bar = True
        self.ops.append(o)

    def mm(self, out, lhsT, rhs, start=True, stop=True):
        self._rec(PE, "matmul", (out, lhsT, rhs), dict(start=start, stop=stop), [lhsT, rhs] + ([] if start else [out]), [out])

    def act(self, out, in_, func, bias=None, scale=None, accum_out=None):
        kw = {}
        reads = [in_]
        if bias is not None:
            kw["bias"] = bias
            reads.append(bias)
        if scale is not None:
            kw["scale"] = scale
            reads.append(scale)
        writes = [out]
        if accum_out is not None:
            kw["accum_out"] = accum_out
            writes.append(accum_out)
        self._rec(ACT, "activation", (out, in_, func), kw, reads, writes)

    def tt(self, out, in0, in1, op, eng=DVE):
        self._rec(eng, "tensor_tensor", (out, in0, in1, op), {}, [in0, in1], [out])

    def ts(self, out, in0, s1, s2, op0, op1=None, eng=DVE):
        kw = {}
        if op1 is not None:
            kw["op1"] = op1
        self._rec(eng, "tensor_scalar", (out, in0, s1, s2, op0), kw, [in0, s1, s2], [out])

    def stt(self, out, in0, scalar, in1, op0, op1):
        self._rec(DVE, "scalar_tensor_tensor", (out, in0, scalar, in1, op0, op1), {}, [in0, scalar, in1], [out])

    def copy(self, out, in_, eng=DVE):
        if eng == ACT:
            self._rec(ACT, "copy", (out, in_), {}, [in_], [out])
        else:
            self._rec(eng, "tensor_copy", (out, in_), {}, [in_], [out])

    def memset(self, out, val, eng=DVE):
        self._rec(eng, "memset", (out, val), {}, [], [out])

    def recip(self, out, in_):
        self._rec(DVE, "reciprocal", (out, in_), {}, [in_], [out])

    def max8(self, out, in_):
        self._rec(DVE, "max", (out, in_), {}, [in_], [out])

    def match_replace(self, out, in_to_replace, in_values, imm):
        self._rec(DVE, "match_replace", (out, in_to_replace, in_values, imm), {}, [in_to_replace, in_values], [out])

    def reduce_sum(self, out, in_, eng=DVE):
        self._rec(eng, "reduce_sum", (out, in_, AX.X), {}, [in_], [out])

    def dma(self, out, in_, eng=SP, grp=None):
        self._rec(eng, "dma_start", (out, in_), {}, [in_], [out], dma=True, grp=grp)

    def emit(self, final_wait_tensors=()):
        nc = self.nc
        ops = self.ops
        last_w = {}
        readers = {}
        last_eng = {}
        dma_since = []
        bar_deps = []
        need_bar = set()
        for i, op in enumerate(ops):
            if op.bar:
                bar_deps = list(last_eng.values()) + dma_since
                dma_since = []
                need_bar = set(self.engs.keys())
                continue
            deps = set()
            for t in op.reads | op.writes:
                for j in last_w.get(t, ()):
                    deps.add(j)
            for t in op.writes:
                for j in readers.get(t, ()):
                    deps.add(j)
            if op.grp is not None:
                deps = {j for j in deps if not (ops[j].grp == op.grp)}
            if op.eng in need_bar:
                deps.update(bar_deps)
                need_bar.discard(op.eng)
            keep = []
            for j in deps:
                oj = ops[j]
                if oj.eng == op.eng and not oj.dma:
                    if op.eng == PE or not self.same_engine_sync:
                        continue
                keep.append(j)
            op.deps = keep
            for j in keep:
                ops[j].sig = True
            for t in op.reads:
                readers.setdefault(t, []).append(i)
            for t in op.writes:
                if op.grp is not None and last_w.get(t) and ops[last_w[t][0]].grp == op.grp:
                    last_w[t].append(i)
                else:
                    last_w[t] = [i]
                readers[t] = []
            if op.dma:
                dma_since.append(i)
            else:
                last_eng[op.eng] = i
        for op in ops:
            if op.dma:
                op.sig = True
        final_ops = []
        for t in final_wait_tensors:
            for j in last_w.get(t, ()):
                ops[j].sig = True
                final_ops.append(j)
        esem = {}
        ecnt = {}
        for e in (PE, ACT, DVE, POOL):
            esem[e] = nc.alloc_semaphore(name="s_" + e)
            ecnt[e] = 0
        dsem = {}
        dcnt = {}
        waited = {e: {} for e in self.engs}
        nwaits = 0
        for i, op in enumerate(ops):
            if op.bar:
                continue
            engobj = self.engs[op.eng]
            need = {}
            for j in op.deps:
                sm, v = ops[j].semval
                if need.get(sm.name, (None, -1))[1] < v:
                    need[sm.name] = (sm, v)
            for nm, (sm, v) in need.items():
                if waited[op.eng].get(nm, -1) >= v:
                    continue
                engobj.wait_ge(sm, v)
                nwaits += 1
                waited[op.eng][nm] = v
            ins = getattr(engobj, op.fn)(*op.args, **op.kw)
            if op.sig:
                if op.dma:
                    key = sorted(op.writes)[0]
                    if key not in dsem:
                        dsem[key] = nc.alloc_semaphore(name="d_" + key)
                        dcnt[key] = 0
                    dcnt[key] += 16
                    ins.then_inc(dsem[key], 16)
                    op.semval = (dsem[key], dcnt[key])
                else:
                    ecnt[op.eng] += 1
                    ins.then_inc(esem[op.eng], 1)
                    op.semval = (esem[op.eng], ecnt[op.eng])
        for j in final_ops:
            s, v = ops[j].semval
            if waited[SP].get(s.name, -1) >= v:
                continue
            nc.sync.wait_ge(s, v)
            waited[SP][s.name] = v
        print(f"[fw] ops={len(ops)} waits={nwaits} " + " ".join(f"{e}={ecnt[e]}" for e in ecnt) + f" dma_sems={len(dsem)}", flush=True)

D = 2048
HD = 128
EPS = 1e-6
NEG = -30000.0
SCALE = HD ** -0.5
NEXP = 32

CHUNKS = ([("T", 0), ("T", 2), ("N", (2, "K", 0)), ("N", (3, "K", 2)), ("V", 0), ("V", 2)]
          + [("N", (5, "K", 4 + 2 * i)) for i in range(4)] + [("V", 4 + 2 * i) for i in range(4)] + [("F", 0)]
          + [("N", (6, "Q", 2 * i)) for i in range(4)] + [("N", (7, "Q", 8 + 2 * i)) for i in range(4)])
NCH = len(CHUNKS)


def build(stop=99, nexp=NEXP, dbg_names=()):
    nc = bass.Bass("TRN2", target_bir_lowering=False)
    k = K(nc)
    used_inputs = []
    dbg_out = []

    def din(name, shape):
        used_inputs.append(name)
        return nc.dram_tensor(name, list(shape), F32, kind="ExternalInput").ap()

    def dscr(name, shape, dt=BF16):
        return nc.dram_tensor(name, list(shape), dt, kind="Internal").ap()

    class Arena:
        def __init__(self, base, size):
            self.base, self.size, self.off = base, size, 0

        def reset(self):
            self.off = 0

        def __call__(self, name, shape, dt=F32):
            nb = int(np.prod(shape[1:])) * (2 if dt == BF16 else 4)
            nb = (nb + 31) // 32 * 32
            assert self.off + nb <= self.size, (name, self.off, nb, self.size)
            t = nc.alloc_sbuf_tensor_at(name, list(shape), dt, offset=self.base + self.off)
            self.off += nb
            return t.ap()

    KB = 1024
    PA = Arena(16 * KB, 44 * KB)
    WA = Arena(60 * KB, 32 * KB)
    B1 = Arena(92 * KB, 64 * KB)
    X2A = Arena(156 * KB, 64 * KB)
    PS = [nc.alloc_psum_tensor(f"ps{i}", [128, 512], F32).ap() for i in range(8)]
    WB = [WA(f"wb{i}", [128, 16, 256], BF16) for i in range(4)]
    cnt = {"w": 0, "p": 0, "t": 0, "o": 0, "e": 0}

    def nxt(key, n):
        v = cnt[key] % n
        cnt[key] += 1
        return v

    def dbg(name, ap):
        if name in dbg_names:
            shp = list(ap.shape)
            o = nc.dram_tensor("dbg_" + name, shp, ap.dtype if hasattr(ap, "dtype") else F32, kind="ExternalOutput").ap()
            k.dma(o, ap, eng=SP)
            dbg_out.append("dbg_" + name)

    def finish():
        k.emit(final_wait_tensors=["out"] + dbg_out if stop >= 12 else dbg_out)
        return nc, used_inputs, dbg_out

    identF = PA("identF", [128, 128]); k.dma(identF, din("identF", [128, 128]))
    identB = PA("identB", [128, 128], BF16); k.copy(identB, identF)
    cT = PA("cT", [128, 16]); k.dma(cT, din("cT", [128, 16]))
    na = PA("na", [128, 16]); k.dma(na, din("na", [128, 16]))
    nm_ = PA("nm", [128, 16]); k.dma(nm_, din("nm", [128, 16]))
    cols = PA("cols", [128, 6, 16])
    A1 = PA("A1", [128, 16]); A2 = PA("A2", [128, 16])
    gAB = PA("gAB", [128, 2048]); gMB = PA("gMB", [128, 2048])
    gB = {2: gAB, 5: gMB}
    fgsb = PA("fgsb", [128, 16, 32])
    small = PA("small", [128, 64])
    ss, rs, rstd = small[:, 0:1], small[:, 1:2], small[:, 2:3]
    ssq, rq, rstdq = small[:, 4:6], small[:, 6:8], small[:, 8:10]
    rc_, fac_ = small[:, 10:12], small[:, 12:14]

    X2A.reset()
    sc_bf = X2A("sc_bf", [128, 16], BF16)
    scB = X2A("scB", [128, 16, 128], BF16)
    k.act(sc_bf, cT, AF.Silu)
    for kt in range(16):
        k.copy(scB[:, kt, :], sc_bf[:, kt:kt + 1].to_broadcast([128, 128]))
    modw = din("modw", [2048, 12288]).rearrange("(kt p) n -> p kt n", p=128)
    modbB = din("modbB", [128, 12288])
    mbs = [X2A(f"mb{i}", [128, 256]) for i in range(2)]
    tmp2 = X2A("tmp2", [128, 256]); tmp3 = X2A("tmp3", [128, 128])
    for ch in range(48):
        v, sub = ch // 8, ch % 8
        wb = WB[nxt("w", 4)]
        k.dma(wb, modw[:, :, ch * 256:(ch + 1) * 256], eng=POOL)
        mb = mbs[ch % 2]
        k.dma(mb, modbB[:, ch * 256:(ch + 1) * 256])
        ps = PS[nxt("p", 4)][:, 0:256]
        for kt in range(16):
            k.mm(ps, scB[:, kt, :], wb[:, kt, :], start=(kt == 0), stop=(kt == 15))
        if v in gB:
            k.tt(gB[v][:, sub * 256:(sub + 1) * 256], ps, mb, ALU.add)
        else:
            k.tt(tmp2, ps, mb, ALU.add)
            for jj in range(2):
                kt = sub * 2 + jj
                k.tt(tmp3, tmp2[:, jj * 128:(jj + 1) * 128], identF, ALU.mult)
                k.reduce_sum(cols[:, v, kt:kt + 1], tmp3)
    k.ts(A1, cols[:, 1, :], 1.0, None, ALU.add); k.tt(A1, A1, na, ALU.mult)
    k.ts(A2, cols[:, 4, :], 1.0, None, ALU.add); k.tt(A2, A2, nm_, ALU.mult)
    Bs1, Bs2 = cols[:, 0, :], cols[:, 3, :]
    dbg("A1", A1); dbg("gAB", gAB)
    if stop <= 1:
        return finish()

    k.barrier()
    X2A.reset(); B1.reset()
    hT = B1("hT", [128, 16, 2048], BF16)
    xts = [X2A(f"xt{i}", [128, 2048]) for i in range(2)]
    junk = X2A("junk", [128, 2048], BF16)
    xn = X2A("xn", [128, 2048], BF16)
    xloc = din("xloc", [2048, 2048])

    def norm_transpose(src, dstT, col0, Acol, Bcol):
        k.act(junk, src, AF.Square, accum_out=ss)
        k.act(rs, ss, AF.Sqrt, bias=EPS, scale=1.0 / D)
        k.recip(rstd, rs)
        k.ts(xn, src, rstd, None, ALU.mult)
        for k4 in range(4):
            pt = PS[4 + nxt("t", 2)]
            for j in range(4):
                kt = k4 * 4 + j
                k.mm(pt[:, j * 128:(j + 1) * 128], xn[:, kt * 128:(kt + 1) * 128], identB)
            for j in range(4):
                kt = k4 * 4 + j
                if j % 2 == 0:
                    k.act(dstT[:, kt, col0:col0 + 128], pt[:, j * 128:(j + 1) * 128], AF.Identity, bias=Bcol[:, kt:kt + 1], scale=Acol[:, kt:kt + 1])
                else:
                    k.ts(dstT[:, kt, col0:col0 + 128], pt[:, j * 128:(j + 1) * 128], Acol[:, kt:kt + 1], Bcol[:, kt:kt + 1], ALU.mult, ALU.add)

    for m in range(16):
        xt = xts[m % 2]
        k.dma(xt, xloc[m * 128:(m + 1) * 128, :])
        norm_transpose(xt, hT, m * 128, A1, Bs1)
    dbg("hT", hT[:, 0, :])
    if stop <= 2:
        return finish()

    k.barrier()
    X2A.reset()
    KTs = dscr("KTs", [12, 128, 2048]); KCr = dscr("KCr", [4, 128, 2048])
    Vs = dscr("Vs", [2048, 12, 128]); QTs = dscr("QTs", [16, 128, 1024])
    gt = X2A("gt", [128, 8, 128])
    gt_in = din("gt", [128, 6, 128])
    k.dma(gt[:, 0:6, :], gt_in)
    k.ts(gt[:, 6, :], gt[:, 0, :], SCALE, None, ALU.mult)
    k.ts(gt[:, 7, :], gt[:, 4, :], SCALE, None, ALU.mult)
    stgs = [X2A(f"stg{i}", [128, 256], BF16) for i in range(4)]
    st2s = [X2A(f"st2{i}", [128, 2, 128], BF16) for i in range(4)]
    junkh = X2A("junkh", [128, 128], BF16)
    winp = din("winp", [2048, NCH * 256]).rearrange("(kt p) n -> p kt n", p=128)
    for ci, (kind, arg) in enumerate(CHUNKS):
        wb = WB[nxt("w", 4)]
        k.dma(wb, winp[:, :, ci * 256:(ci + 1) * 256], eng=POOL)
        tiles = range(16) if ci < 15 else range(8, 16)
        for m in tiles:
            ps = PS[nxt("p", 4)][:, 0:256]
            for kt in range(16):
                k.mm(ps, hT[:, kt, m * 128:(m + 1) * 128], wb[:, kt, :], start=(kt == 0), stop=(kt == 15))
            if kind == "F":
                k.copy(fgsb[:, m, :], ps[:, 0:32], eng=ACT)
                continue
            stg = stgs[nxt("e", 4)]
            if kind == "V":
                k.copy(stg, ps, eng=ACT)
                k.dma(Vs[m * 128:(m + 1) * 128, arg:arg + 2, :], stg.rearrange("p (j d) -> p j d", j=2), grp="s3")
                continue
            if kind == "T":
                k.copy(stg, ps, eng=ACT)
            else:
                gi = arg[0]
                for j in range(2):
                    k.act(junkh, ps[:, j * 128:(j + 1) * 128], AF.Square, accum_out=ssq[:, j:j + 1])
                k.act(rq, ssq, AF.Sqrt, bias=EPS, scale=1.0 / HD)
                k.recip(rstdq, rq)
                for j in range(2):
                    k.stt(stg[:, j * 128:(j + 1) * 128], ps[:, j * 128:(j + 1) * 128], rstdq[:, j:j + 1], gt[:, gi, :], ALU.mult, ALU.mult)
            pt = PS[4 + nxt("t", 2)]
            for j in range(2):
                k.mm(pt[:, j * 128:(j + 1) * 128], stg[:, j * 128:(j + 1) * 128], identB)
            st2 = st2s[nxt("o", 4)]
            k.copy(st2, pt[:, 0:256].rearrange("p (j t) -> p j t", j=2))
            if kind == "T":
                dst = KCr[arg:arg + 2, :, m * 128:(m + 1) * 128]
            elif arg[1] == "K":
                dst = KTs[arg[2]:arg[2] + 2, :, m * 128:(m + 1) * 128]
            else:
                dst = QTs[arg[2]:arg[2] + 2, :, (m - 8) * 128:(m - 7) * 128]
            k.dma(dst.rearrange("j d t -> d j t"), st2, grp="s3")
    if "QTs" in dbg_names:
        qd = X2A("qd", [128, 1024], BF16); k.dma(qd, QTs[0]); dbg("QTs", qd)
        kd = X2A("kd", [128, 2048], BF16); k.dma(kd, KTs[4]); dbg("KTs", kd)
    dbg("fgsb", fgsb)
    if stop <= 3:
        return finish()

    k.barrier()
    X2A.reset(); B1.reset()
    def cload(ar, name, shape, dt=F32, eng=SP, src=None):
        t = ar(name, shape, dt)
        k.dma(t, src if src is not None else din(name, shape), eng=(POOL if dt == BF16 else eng))
        return t
    triF = cload(X2A, "triF", [128, 128]); onesF = cload(X2A, "onesF", [128, 128]); e64 = cload(X2A, "e64", [128, 128])
    fbias = cload(X2A, "fbias", [128, 8]); gbias = cload(X2A, "gbias", [128, 24])
    padk8 = cload(PA, "padk8", [128, 16, 8]); farb = cload(X2A, "farb", [128, 8])
    padn = cload(PA, "padn", [128, 1])
    padfar = PA("padfar", [128, 16, 8]); biasF = PA("biasF", [128, 8, 16, 8]); gsig = PA("gsig", [128, 8, 24])
    fl = X2A("fl", [128, 16, 8]); sg_ = X2A("sg_", [128, 128]); ls = X2A("ls", [128, 128])
    wsb = X2A("wsb", [128, 128]); tot = X2A("tot", [128, 16, 8]); offs = X2A("offs", [128, 16, 8])
    cum = X2A("cum", [128, 16, 8]); crefs = X2A("crefs", [128, 16, 8]); cumpad = X2A("cumpad", [128, 16, 8])
    for m in range(16):
        k.tt(fl[:, m, :], fgsb[:, m, 0:8], fbias, ALU.add)
        k.tt(padfar[:, m, :], padk8[:, m, :], farb, ALU.add)
    flf = fl.rearrange("p m h -> p (m h)")
    k.act(sg_, flf, AF.Sigmoid); k.act(ls, sg_, AF.Ln)
    k.mm(PS[0][:, 0:128], triF, ls); k.mm(PS[1][:, 0:128], onesF, ls)
    k.copy(wsb, PS[0][:, 0:128]); k.copy(tot.rearrange("p m h -> p (m h)"), PS[1][:, 0:128])
    k.memset(offs[:, 0, :], 0.0)
    for m in range(1, 16):
        k.tt(offs[:, m, :], offs[:, m - 1, :], tot[:, m - 1, :], ALU.add)
    cumf = cum.rearrange("p m h -> p (m h)")
    k.tt(cumf, wsb, offs.rearrange("p m h -> p (m h)"), ALU.add)
    k.mm(PS[2][:, 0:128], e64, cumf)
    k.copy(crefs.rearrange("p m h -> p (m h)"), PS[2][:, 0:128])
    k.tt(cumpad, cum, padk8, ALU.subtract)
    for i in range(8):
        for kb in range(9 + i):
            k.tt(biasF[:, i, kb, :], crefs[:, 8 + i, :], cumpad[:, kb, :], ALU.subtract)
        k.tt(gsig[:, i, :], fgsb[:, 8 + i, 8:32], gbias, ALU.add)
    k.act(gsig.rearrange("p i g -> p (i g)"), gsig.rearrange("p i g -> p (i g)"), AF.Sigmoid)
    dbg("biasF", biasF.rearrange("p i k h -> p (i k h)")); dbg("gsig", gsig.rearrange("p i g -> p (i g)"))
    if stop <= 4:
        return finish()

    k.barrier()
    X2A.reset()
    oacc = X2A("oacc", [128, 8, 1024])
    B1.reset()
    AT = Arena(B1.base, 32 * KB)
    mixT = Arena(B1.base + 32 * KB, 32 * KB)("mixT", [128, 16, 1024], BF16)
    XT = Arena(X2A.base + 32 * KB, 32 * KB)
    gtc = XT("gtc", [128, 6, 128]); k.dma(gtc, gt_in)
    KCT = [PA(f"KCT{g}", [128, 127], BF16) for g in range(2)]
    VCa = [PA(f"VCa{g}", [128, 161], BF16) for g in range(2)]
    ovl_in = din("ovl", [127, 32])
    for g in range(2):
        k.memset(VCa[g][:, 128:129], 1.0)
        k.dma(VCa[g][0:127, 129:161], ovl_in, eng=POOL)
    posT = cload(XT, "posT", [128, 2, 32])
    w1_in = din("w1", [2, 4096, 256]); w2_in = din("w2", [2, 256, 128])
    w1sb = AT("w1sb", [128, 32, 256], BF16); w2sb = AT("w2sb", [128, 2, 128], BF16)
    kcraw = AT("kcraw", [128, 2048], BF16); tmpl = AT("tmpl", [128, 32, 127], BF16)
    GT = [AT(f"GT{i}", [128, 127], BF16) for i in range(2)]
    gx2 = XT("gx2", [128, 127]); gu = XT("gu", [128, 127]); gth = XT("gth", [128, 127])
    kcn = XT("kcn", [128, 128], BF16); junkc = XT("junkc", [128, 128], BF16)
    for kv in range(2):
        k.dma(w1sb, w1_in[kv].rearrange("(l d) h -> d l h", d=128), eng=POOL)
        k.dma(w2sb, w2_in[kv].rearrange("(hh p) d -> p hh d", p=128), eng=POOL)
        for g in range(2):
            k.dma(kcraw, KCr[kv * 2 + g])
            for l in range(32):
                k.ts(tmpl[:, l, :], kcraw[:, l:l + 2017:16], posT[:, kv, l:l + 1], None, ALU.add)
            for hh in range(2):
                ps = PS[hh][:, 0:127]
                for l in range(32):
                    k.mm(ps, w1sb[:, l, hh * 128:(hh + 1) * 128], tmpl[:, l, :], start=(l == 0), stop=(l == 31))
                k.act(gx2, ps, AF.Square)
                k.ts(gu, gx2, 0.044715, 1.0, ALU.mult, ALU.add)
                k.tt(gu, gu, ps, ALU.mult)
                k.act(gth, gu, AF.Tanh, scale=0.7978845608028654)
                k.ts(gth, gth, 0.5, 0.5, ALU.mult, ALU.add)
                k.tt(GT[hh], gth, ps, ALU.mult)
            pk = PS[2][0:127, 0:128]
            k.mm(pk, GT[0], w2sb[:, 0, :], start=True, stop=False)
            k.mm(pk, GT[1], w2sb[:, 1, :], start=False, stop=True)
            if kv == 0:
                k.act(junkc[0:127, :], pk, AF.Square, accum_out=ss[0:127, :])
                k.act(rs[0:127, :], ss[0:127, :], AF.Sqrt, bias=EPS, scale=1.0 / HD)
                k.recip(rstd[0:127, :], rs[0:127, :])
                k.stt(kcn[0:127, :], pk, rstd[0:127, :], gtc[0:127, 1, :], ALU.mult, ALU.mult)
                k.mm(PS[4][:, 0:127], kcn[0:127, :], identB[0:127, 0:127])
                k.copy(KCT[g], PS[4][:, 0:127])
            else:
                k.copy(VCa[g][0:127, 0:128], pk)
    dbg("KCT0", KCT[0]); dbg("VCa0", VCa[0])
    if stop <= 5:
        return finish()

    k.barrier()
    AT.reset(); XT.reset()
    qTs = [AT(f"qT{i}", [128, 1024], BF16) for i in range(2)]
    bcs = [AT(f"bc{i}", [128, 1024], BF16) for i in range(2)]
    pTs = [AT(f"pT{i}", [128, 512], BF16) for i in range(4)]
    vAs = [AT(f"vA{i}", [128, 16, 129], BF16) for i in range(2)]
    for v_ in vAs:
        k.memset(v_[:, :, 128:129], 1.0)
    negselT = [AT(f"negsel{g}", [32, 1024], BF16) for g in range(2)]
    t01 = cload(AT, "t01", [128, 8, 2, 128], BF16)
    wm4 = cload(AT, "wm4", [128, 128], BF16); causalB = cload(AT, "causal", [128, 128], BF16)
    imp = XT("imp", [128, 8, 32]); vmul = cload(XT, "vmul", [128, 8, 32]); amask = cload(XT, "amask", [128, 8, 32])
    impm = XT("impm", [128, 32]); wk_ = XT("wk_", [128, 32]); m8 = XT("m8", [128, 16]); nsel = XT("nsel", [128, 32], BF16)
    onormB = cload(XT, "onormB", [128, 2048])
    kTs = [XT(f"kT{i}", [128, 2048], BF16) for i in range(2)]
    ekb = cload(XT, "ekb", [32, 16, 128], BF16)
    mix = XT("mix", [128, 1024], BF16); junko = XT("junko", [128, 1024], BF16)
    biascmp_in = din("biascmp", [8, 127, 1024])
    qcnt = [0]

    def load_q(hq):
        q = qTs[qcnt[0] % 2]; qcnt[0] += 1
        k.dma(q, QTs[hq])
        return q

    for g in range(2):
        for r in range(4):
            h = 4 * g + r
            qT = load_q(h)
            bc = bcs[h % 2]
            k.dma(bc[0:127, :], biascmp_in[h], eng=POOL)
            for half in range(2):
                ps = PS[nxt("p", 4)][0:127, :]
                k.mm(ps, KCT[g], qT[:, half * 512:(half + 1) * 512], start=True, stop=False)
                k.mm(ps, identB[0:127, 0:127], bc[0:127, half * 512:(half + 1) * 512], start=False, stop=True)
                pT = pTs[nxt("e", 4)]
                k.act(pT[0:127, :], ps, AF.Exp, bias=padn[0:127, :])
                for i4 in range(4):
                    i = half * 4 + i4
                    po = PS[6 + nxt("o", 2)][:, 0:161]
                    k.mm(po, pT[0:127, i4 * 128:(i4 + 1) * 128], VCa[g][0:127, :])
                    k.ts(rc_[:, 1:2], po[:, 128:129], 1e-30, None, ALU.max)
                    k.recip(rc_[:, 0:1], rc_[:, 1:2])
                    k.tt(fac_[:, 0:1], rc_[:, 0:1], gsig[:, i, h:h + 1], ALU.mult)
                    k.ts(oacc[:, i, h * 128:(h + 1) * 128], po[:, 0:128], fac_[:, 0:1], None, ALU.mult)
                    if r == 0:
                        k.ts(imp[:, i, :], po[:, 129:161], rc_[:, 0:1], None, ALU.mult)
                    else:
                        k.stt(imp[:, i, :], po[:, 129:161], rc_[:, 0:1], imp[:, i, :], ALU.mult, ALU.add)
        for i in range(8):
            k.tt(impm, imp[:, i, :], vmul[:, i, :], ALU.mult)
            k.tt(impm, impm, amask[:, i, :], ALU.add)
            k.max8(m8[:, 0:8], impm)
            k.match_replace(wk_, m8[:, 0:8], impm, -1e9)
            k.max8(m8[:, 8:16], wk_)
            k.ts(nsel, impm, m8[:, 15:16], -NEG, ALU.is_ge, ALU.mult)
            k.ts(nsel, nsel, NEG, None, ALU.add)
            k.mm(PS[4][0:32, 0:128], nsel, identB)
            k.copy(negselT[g][:, i * 128:(i + 1) * 128], PS[4][0:32, 0:128])
    dbg("oacc_cmp", oacc[:, 0, :]); dbg("negsel0", negselT[0])
    if stop <= 6:
        return finish()

    kvc = [0]

    def load_kv(idx):
        kT = kTs[kvc[0] % 2]; vA = vAs[kvc[0] % 2]; kvc[0] += 1
        k.dma(kT, KTs[idx])
        k.dma(vA[:, :, 0:128], Vs[:, idx, :].rearrange("(m p) d -> p m d", p=128))
        return kT, vA

    def attend(kT, vA, qT, h, i, kbs, mode, g):
        qb = 8 + i
        po = PS[6 + nxt("o", 2)][:, 0:129]
        for kb in kbs:
            dl = qb - kb
            ps = PS[nxt("p", 4)][:, 0:128]
            extra = []
            if mode == "fox":
                if dl == 0:
                    extra.append((identB, causalB))
                bias = biasF[:, i, kb, h:h + 1]
            else:
                if dl <= 1:
                    extra.append((identB, t01[:, h, dl, :]))
                    bias = padk8[:, kb, 0:1]
                else:
                    bias = padfar[:, kb, h:h + 1]
                    if mode == "win" and dl == 4:
                        extra.append((identB, wm4))
                if mode == "slc":
                    extra.append((ekb[:, kb, :], negselT[g][:, i * 128:(i + 1) * 128]))
            k.mm(ps, kT[:, kb * 128:(kb + 1) * 128], qT[:, i * 128:(i + 1) * 128], start=True, stop=(len(extra) == 0))
            for xi, (l_, r_) in enumerate(extra):
                k.mm(ps, l_, r_, start=False, stop=(xi == len(extra) - 1))
            pT = pTs[nxt("e", 4)][:, 0:128]
            k.act(pT, ps, AF.Exp, bias=bias)
            k.mm(po, pT, vA[:, kb, :], start=(kb == kbs[0]), stop=(kb == kbs[-1]))
        return po

    for g in range(2):
        for mode, kidx, goff in (("slc", g, 8), ("win", 2 + g, 16)):
            kT, vA = load_kv(kidx)
            for r in range(4):
                h = 4 * g + r
                qT = load_q(h)
                for i in range(8):
                    qb = 8 + i
                    kbs = list(range(qb + 1)) if mode == "slc" else list(range(qb - 4, qb + 1))
                    po = attend(kT, vA, qT, h, i, kbs, mode, g)
                    k.recip(rc_[:, 0:1], po[:, 128:129])
                    k.tt(fac_[:, 0:1], rc_[:, 0:1], gsig[:, i, goff + h:goff + h + 1], ALU.mult)
                    k.stt(oacc[:, i, h * 128:(h + 1) * 128], po[:, 0:128], fac_[:, 0:1], oacc[:, i, h * 128:(h + 1) * 128], ALU.mult, ALU.add)

    def out_norm(koff):
        for i in range(8):
            k.act(junko, oacc[:, i, :], AF.Square, accum_out=ss)
            k.act(rs, ss, AF.Sqrt, bias=EPS, scale=1.0 / 1024)
            k.recip(rstd, rs)
            k.stt(mix, oacc[:, i, :], rstd, onormB[:, koff * 128:koff * 128 + 1024], ALU.mult, ALU.mult)
            for k4 in range(2):
                pt = PS[4 + nxt("t", 2)]
                for j in range(4):
                    kt = k4 * 4 + j
                    k.mm(pt[:, j * 128:(j + 1) * 128], mix[:, kt * 128:(kt + 1) * 128], identB)
                k.copy(mixT[:, koff + k4 * 4:koff + k4 * 4 + 4, i * 128:(i + 1) * 128], pt.rearrange("p (j t) -> p j t", j=4), eng=(ACT if k4 == 0 else DVE))

    dbg("oacc_nsa", oacc[:, 0, :])
    out_norm(0)
    if stop <= 7:
        dbg("mixT", mixT[:, 0, :])
        return finish()
    for h in range(8):
        kT, vA = load_kv(4 + h)
        qT = load_q(8 + h)
        for i in range(8):
            po = attend(kT, vA, qT, h, i, list(range(9 + i)), "fox", 0)
            k.recip(rc_[:, 0:1], po[:, 128:129])
            k.ts(oacc[:, i, h * 128:(h + 1) * 128], po[:, 0:128], rc_[:, 0:1], None, ALU.mult)
    dbg("oacc_fox", oacc[:, 0, :])
    out_norm(8)
    dbg("mixT", mixT[:, 8, :])
    if stop <= 8:
        return finish()

    k.barrier()
    X2A.reset(); AT.reset()
    x2 = X2A("x2", [128, 8, 2048])
    for i in range(8):
        k.dma(x2[:, i, :], xloc[(8 + i) * 128:(9 + i) * 128, :])
    tmps = [AT(f"tmpo{i}", [128, 512]) for i in range(3)]
    wout = din("wout", [2048, 2048]).rearrange("(kt p) n -> p kt n", p=128)
    for c in range(8):
        wb = WB[nxt("w", 4)]
        k.dma(wb, wout[:, :, c * 256:(c + 1) * 256], eng=POOL)
        for i in range(8):
            ps = PS[nxt("p", 4)][:, 0:256]
            for kt in range(16):
                k.mm(ps, mixT[:, kt, i * 128:(i + 1) * 128], wb[:, kt, :], start=(kt == 0), stop=(kt == 15))
            tmp = tmps[nxt("e", 3)][:, 0:256]
            k.tt(tmp, ps, gAB[:, c * 256:(c + 1) * 256], ALU.mult)
            k.tt(x2[:, i, c * 256:(c + 1) * 256], x2[:, i, c * 256:(c + 1) * 256], tmp, ALU.add)
    dbg("x2", x2[:, 0, :])
    if stop <= 9:
        return finish()

    k.barrier()
    B1.reset()
    h2T = B1("h2T", [128, 16, 1024], BF16)
    actT = B1("actT", [128, 16, 1024], BF16)
    MT = Arena(B1.base + 32 * KB, 32 * KB)
    junk2 = MT("junk2", [128, 2048], BF16); xn2 = MT("xn2", [128, 2048], BF16)
    junk, xn = junk2, xn2
    def norm_transpose2(src, dstT, col0, Acol, Bcol):
        k.act(junk2, src, AF.Square, accum_out=ss)
        k.act(rs, ss, AF.Sqrt, bias=EPS, scale=1.0 / D)
        k.recip(rstd, rs)
        k.ts(xn2, src, rstd, None, ALU.mult)
        for k4 in range(4):
            pt = PS[4 + nxt("t", 2)]
            for j in range(4):
                kt = k4 * 4 + j
                k.mm(pt[:, j * 128:(j + 1) * 128], xn2[:, kt * 128:(kt + 1) * 128], identB)
            for j in range(4):
                kt = k4 * 4 + j
                if j % 2 == 0:
                    k.act(dstT[:, kt, col0:col0 + 128], pt[:, j * 128:(j + 1) * 128], AF.Identity, bias=Bcol[:, kt:kt + 1], scale=Acol[:, kt:kt + 1])
                else:
                    k.ts(dstT[:, kt, col0:col0 + 128], pt[:, j * 128:(j + 1) * 128], Acol[:, kt:kt + 1], Bcol[:, kt:kt + 1], ALU.mult, ALU.add)
    for i in range(8):
        norm_transpose2(x2[:, i, :], h2T, i * 128, A2, Bs2)
    rwsb = MT("rwsb", [128, 16, 32], BF16)
    k.dma(rwsb, din("rw", [2048, 32]).rearrange("(kt p) e -> p kt e", p=128), eng=POOL)
    rbB = cload(MT, "rbB", [128, 32])
    gates = PA("gates", [128, 8, 32]); bupT = cload(PA, "bupT", [128, 32, 32])
    lg = MT("lg", [128, 32]); m8r = MT("m8r", [128, 8]); nmx = MT("nmx", [128, 1]); msk = MT("msk", [128, 32])
    ex = MT("ex", [128, 32]); rsm = MT("rsm", [128, 1]); rrm = MT("rrm", [128, 1])
    gTs = MT("gTs", [32, 128]); bdn = cload(MT, "bdn", [32, 2048])
    tmpb = [MT(f"tmpb{i}", [128, 512]) for i in range(2)]
    for i in range(8):
        ps = PS[nxt("p", 4)][:, 0:32]
        for kt in range(16):
            k.mm(ps, h2T[:, kt, i * 128:(i + 1) * 128], rwsb[:, kt, :], start=(kt == 0), stop=(kt == 15))
        k.tt(lg, ps, rbB, ALU.add)
        k.max8(m8r, lg)
        k.ts(nmx, m8r[:, 0:1], -1.0, None, ALU.mult)
        k.ts(msk, lg, m8r[:, 3:4], None, ALU.is_ge)
        k.act(ex, lg, AF.Exp, bias=nmx)
        k.tt(ex, ex, msk, ALU.mult)
        k.reduce_sum(rsm, ex)
        k.recip(rrm, rsm)
        k.ts(gates[:, i, :], ex, rrm, None, ALU.mult)
        k.mm(PS[4][0:32, 0:128], gates[:, i, :], identF)
        k.copy(gTs, PS[4][0:32, 0:128])
        for c in range(4):
            pb = PS[nxt("p", 4)]
            k.mm(pb, gTs, bdn[:, c * 512:(c + 1) * 512])
            tb = tmpb[c % 2]
            k.tt(tb, pb, gMB[:, c * 512:(c + 1) * 512], ALU.mult)
            k.tt(x2[:, i, c * 512:(c + 1) * 512], x2[:, i, c * 512:(c + 1) * 512], tb, ALU.add)
    dbg("gates", gates.rearrange("p i e -> p (i e)")); dbg("h2T", h2T[:, 0, :])
    if stop <= 10:
        return finish()

    k.barrier()
    ET = Arena(PA.base + PA.off, PA.size - PA.off)
    tg = ET("tg", [128, 512]); tsg = ET("tsg", [128, 512]); tl = ET("tl", [128, 512]); ta = ET("ta", [128, 512])
    tds = [ET(f"td{i}", [128, 256]) for i in range(2)]
    wup = din("wup", [NEXP, 2048, 4096]); wdn = din("wdn", [NEXP, 2048, 2048])
    pset = [0]
    for e in range(nexp):
        wu = wup[e].rearrange("(kt p) f -> p kt f", p=128)
        for c in range(8):
            wg = WB[nxt("w", 4)]; k.dma(wg, wu[:, :, c * 256:(c + 1) * 256], eng=POOL)
            wl = WB[nxt("w", 4)]; k.dma(wl, wu[:, :, 2048 + c * 256:2048 + (c + 1) * 256], eng=POOL)
            for j in range(2):
                ft = c * 2 + j
                base = 4 * (pset[0] % 2); pset[0] += 1
                for kt in range(16):
                    for tc in range(2):
                        k.mm(PS[base + tc], wg[:, kt, j * 128:(j + 1) * 128], h2T[:, kt, tc * 512:(tc + 1) * 512], start=(kt == 0), stop=(kt == 15))
                for kt in range(16):
                    for tc in range(2):
                        k.mm(PS[base + 2 + tc], wl[:, kt, j * 128:(j + 1) * 128], h2T[:, kt, tc * 512:(tc + 1) * 512], start=(kt == 0), stop=(kt == 15))
                for tc in range(2):
                    k.ts(tg, PS[base + tc], bupT[:, e, ft:ft + 1], 7.0, ALU.add, ALU.min)
                    k.act(tsg, tg, AF.Sigmoid, scale=1.702)
                    k.ts(tl, PS[base + 2 + tc], bupT[:, e, 16 + ft:17 + ft], 7.0, ALU.add, ALU.min)
                    k.ts(tl, tl, -7.0, 1.0, ALU.max, ALU.add)
                    k.tt(ta, tg, tsg, ALU.mult)
                    k.tt(actT[:, ft, tc * 512:(tc + 1) * 512], ta, tl, ALU.mult)
        wd_ = wdn[e].rearrange("(kt p) d -> p kt d", p=128)
        for c in range(8):
            wd = WB[nxt("w", 4)]; k.dma(wd, wd_[:, :, c * 256:(c + 1) * 256], eng=POOL)
            for i in range(8):
                ps = PS[nxt("p", 8)][:, 0:256]
                for kt in range(16):
                    k.mm(ps, actT[:, kt, i * 128:(i + 1) * 128], wd[:, kt, :], start=(kt == 0), stop=(kt == 15))
                td = tds[nxt("e", 2)]
                k.tt(td, ps, gMB[:, c * 256:(c + 1) * 256], ALU.mult)
                k.stt(x2[:, i, c * 256:(c + 1) * 256], td, gates[:, i, e:e + 1], x2[:, i, c * 256:(c + 1) * 256], ALU.mult, ALU.add)

    outd = nc.dram_tensor("out", [1024, 2048], F32, kind="ExternalOutput").ap()
    for i in range(8):
        k.dma(outd[i * 128:(i + 1) * 128, :], x2[:, i, :])
    stop = 12
    return finish()


def _t5_bucket(dist):
    n = np.maximum(dist, 0)
    nf = np.maximum(n, 1).astype(np.float32)
    large = 16 + (np.log(nf / np.float32(16)) / np.float32(math.log(8.0)) * np.float32(16)).astype(np.int32)
    large = np.minimum(large, 31)
    return np.where(n < 16, n, large)


def host_prep(inp, core):
    f32 = np.float32
    b, p = core // 2, core % 2
    x = np.asarray(inp["x"], f32)
    o = {}
    if p == 1:
        o["xloc"] = np.ascontiguousarray(x[b])
    else:
        o["xloc"] = np.concatenate([np.zeros((1024, D), f32), x[b, :1024]], axis=0)
    col = lambda v: np.ascontiguousarray(np.asarray(v, f32).reshape(16, 128).T)
    rep = lambda v: np.ascontiguousarray(np.broadcast_to(np.asarray(v, f32).reshape(1, -1), (128, np.asarray(v).size)))
    o["cT"] = col(inp["c"][b])
    o["na"] = col(inp["norm_attn"][0]); o["nm"] = col(inp["norm_moe"][0])
    o["modw"] = np.asarray(inp["mod_w"][0], f32)
    o["modbB"] = rep(inp["mod_b"][0])
    o["identF"] = np.eye(128, dtype=f32)
    w = np.asarray(inp["w_in"][0], f32)
    sizes = [1024, 256, 256, 256, 256, 256, 256, 24, 1024, 1024, 1024, 8]
    offs = np.concatenate([[0], np.cumsum(sizes)])
    seg = lambda i: w[:, offs[i]:offs[i + 1]]
    fcols = np.zeros((D, 256), f32)
    fcols[:, 0:8] = seg(11); fcols[:, 8:32] = seg(7)
    o["winp"] = np.ascontiguousarray(np.concatenate(
        [seg(1), seg(2), seg(3), seg(5), seg(4), seg(6), seg(9), seg(10), fcols, seg(0), seg(8)], axis=1))
    kn = np.asarray(inp["nsa_k_norm"][0], f32)
    gts = [inp["nsa_q_norm"][0], kn[0], kn[1], kn[2], inp["fox_q_norm"][0], inp["fox_k_norm"][0]]
    o["gt"] = np.ascontiguousarray(np.stack([rep(g) for g in gts], axis=1))
    o["fbias"] = rep(inp["fox_forget_bias"][0]); o["gbias"] = rep(inp["b_nsa_gate"][0])
    o["posT"] = np.ascontiguousarray(np.stack([np.asarray(inp["cmp_pos_k"][0], f32).T, np.asarray(inp["cmp_pos_v"][0], f32).T], axis=1))
    o["w1"] = np.ascontiguousarray(np.stack([inp["cmp_k_w1"][0], inp["cmp_v_w1"][0]]).astype(f32))
    o["w2"] = np.ascontiguousarray(np.stack([inp["cmp_k_w2"][0], inp["cmp_v_w2"][0]]).astype(f32))
    n = np.arange(127)[:, None]; j = np.arange(32)[None, :]
    o["ovl"] = ((16 * n < 64 * j + 64) & (16 * n + 32 > 64 * j)).astype(f32)
    rb = np.asarray(inp["rel_bias"], f32)
    t = np.arange(1024)[None, :]
    d = (1024 + t) - (16 * n + 31)
    bc = rb[_t5_bucket(d)]
    bc = np.where((d >= 0)[..., None], bc, f32(NEG))
    o["biascmp"] = np.ascontiguousarray(bc.transpose(2, 0, 1).astype(f32))
    s = np.arange(128)[:, None]; tt_ = np.arange(128)[None, :]
    t01 = np.zeros((128, 8, 2, 128), f32)
    for dl in range(2):
        dd = dl * 128 + tt_ - s
        v = rb[_t5_bucket(dd)]
        v = np.where((dd >= 0)[..., None], v, f32(NEG))
        t01[:, :, dl, :] = v.transpose(0, 2, 1)
    o["t01"] = t01
    o["farb"] = rep(rb[31])
    o["wm4"] = np.where(s > tt_, f32(0), f32(NEG)).astype(f32)
    o["causal"] = np.where(s <= tt_, f32(0), f32(NEG)).astype(f32)
    sl = np.arange(128)[:, None] + 128 * np.arange(16)[None, :]
    padk = np.where((sl < 1024) & (p == 0), f32(NEG), f32(0)).astype(f32)
    o["padk8"] = np.ascontiguousarray(np.broadcast_to(padk[:, :, None], (128, 16, 8)))
    pn = np.zeros((128, 1), f32)
    if p == 0:
        pn[:64] = NEG
    o["padn"] = pn
    tl = (1024 + np.arange(1024)).reshape(8, 128).T
    treal = tl - 1024 * (1 - p)
    cur = (treal // 64)[:, :, None]
    jr = (np.arange(32) - 16 * (1 - p))[None, None, :]
    valid = (jr >= 0) & (jr <= cur)
    f0 = valid & (jr == 0); f1 = valid & (jr == cur) & ~f0; f2 = valid & (jr == cur - 1) & ~f0 & ~f1
    am = np.where(f0, 3e6, np.where(f1, 2e6, np.where(f2, 1e6, np.where(valid, 0.0, -1e6))))
    o["amask"] = am.astype(f32)
    o["vmul"] = (valid & ~f0 & ~f1 & ~f2).astype(f32)
    ek = np.zeros((32, 16, 128), f32)
    for kb in range(16):
        ek[2 * kb, kb, :64] = 1; ek[2 * kb + 1, kb, 64:] = 1
    o["ekb"] = ek
    o["triF"] = (np.arange(128)[:, None] <= np.arange(128)[None, :]).astype(f32)
    o["onesF"] = np.ones((128, 128), f32)
    e64 = np.zeros((128, 128), f32); e64[64, :] = 1
    o["e64"] = e64
    o["onormB"] = rep(inp["out_norm"][0])
    o["wout"] = np.asarray(inp["w_out"][0], f32)
    o["rw"] = np.asarray(inp["router_w"][0], f32)
    o["rbB"] = rep(inp["router_b"][0])
    o["wup"] = np.asarray(inp["exp_w_up"][0], f32)
    o["wdn"] = np.asarray(inp["exp_w_down"][0], f32)
    bu = np.asarray(inp["exp_b_up"][0], f32)
    o["bupT"] = np.ascontiguousarray(bu.reshape(32, 32, 128).transpose(2, 0, 1))
    o["bdn"] = np.asarray(inp["exp_b_down"][0], f32)
    return o


_CACHE = {}


def kernel(**inputs):
    if "prog" not in _CACHE:
        _CACHE["prog"] = build()
    nc, used, _ = _CACHE["prog"]
    in_maps = []
    for core in range(8):
        hp = host_prep(inputs, core)
        in_maps.append({n: hp[n] for n in used})
    res = run_bass_kernel_spmd(nc, in_maps, core_ids=list(range(8)))
    out = np.zeros((4, 2048, D), np.float32)
    for core in range(8):
        b, p = core // 2, core % 2
        out[b, p * 1024:(p + 1) * 1024] = res.results[core]["out"]
    return out
```

```python
import math
import numpy as np
import concourse.bass as bass
import concourse.mybir as mybir
from concourse.bass_utils import run_bass_kernel_spmd

F32 = mybir.dt.float32
BF16 = mybir.dt.bfloat16
ALU = mybir.AluOpType
AF = mybir.ActivationFunctionType
AX = mybir.AxisListType
PE, ACT, DVE, POOL, SP = "pe", "act", "dve", "pool", "sp"


def _tname(ap):
    t = getattr(ap, "tensor", None)
    if t is None:
        t = ap
    return t.name


class Op:
    __slots__ = ("eng", "fn", "args", "kw", "reads", "writes", "dma", "deps", "sig", "semval", "grp", "bar")

    def __init__(self, eng, fn, args, kw, reads, writes, dma=False, grp=None):
        self.eng = eng
        self.fn = fn
        self.args = args
        self.kw = kw
        self.reads = reads
        self.writes = writes
        self.dma = dma
        self.deps = []
        self.sig = False
        self.semval = None
        self.grp = grp
        self.bar = False


class K:
    def __init__(self, nc, same_engine_sync=True):
        self.nc = nc
        self.ops = []
        self.same_engine_sync = same_engine_sync
        self.engs = {PE: nc.tensor, ACT: nc.scalar, DVE: nc.vector, POOL: nc.gpsimd, SP: nc.sync}

    def _rec(self, eng, fn, args, kw, reads, writes, dma=False, grp=None):
        r = set(_tname(a) for a in reads if a is not None and not isinstance(a, (int, float)))
        w = set(_tname(a) for a in writes)
        self.ops.append(Op(eng, fn, args, kw, r, w, dma, grp))

    def barrier(self):
        o = Op(None, None, None, None, set(), set())
        o.bar = True
        self.ops.append(o)

    def mm(self, out, lhsT, rhs, start=True, stop=True):
        self._rec(PE, "matmul", (out, lhsT, rhs), dict(start=start, stop=stop), [lhsT, rhs] + ([] if start else [out]), [out])

    def act(self, out, in_, func, bias=None, scale=None, accum_out=None):
        kw = {}
        reads = [in_]
        if bias is not None:
            kw["bias"] = bias
            reads.append(bias)
        if scale is not None:
            kw["scale"] = scale
            reads.append(scale)
        writes = [out]
        if accum_out is not None:
            kw["accum_out"] = accum_out
            writes.append(accum_out)
        self._rec(ACT, "activation", (out, in_, func), kw, reads, writes)

    def tt(self, out, in0, in1, op, eng=DVE):
        self._rec(eng, "tensor_tensor", (out, in0, in1, op), {}, [in0, in1], [out])

    def ts(self, out, in0, s1, s2, op0, op1=None, eng=DVE):
        kw = {}
        if op1 is not None:
            kw["op1"] = op1
        self._rec(eng, "tensor_scalar", (out, in0, s1, s2, op0), kw, [in0, s1, s2], [out])

    def stt(self, out, in0, scalar, in1, op0, op1):
        self._rec(DVE, "scalar_tensor_tensor", (out, in0, scalar, in1, op0, op1), {}, [in0, scalar, in1], [out])

    def copy(self, out, in_, eng=DVE):
        if eng == ACT:
            self._rec(ACT, "copy", (out, in_), {}, [in_], [out])
        else:
            self._rec(eng, "tensor_copy", (out, in_), {}, [in_], [out])

    def memset(self, out, val, eng=DVE):
        self._rec(eng, "memset", (out, val), {}, [], [out])

    def recip(self, out, in_):
        self._rec(DVE, "reciprocal", (out, in_), {}, [in_], [out])

    def max8(self, out, in_):
        self._rec(DVE, "max", (out, in_), {}, [in_], [out])

    def match_replace(self, out, in_to_replace, in_values, imm):
        self._rec(DVE, "match_replace", (out, in_to_replace, in_values, imm), {}, [in_to_replace, in_values], [out])

    def reduce_sum(self, out, in_, eng=DVE):
        self._rec(eng, "reduce_sum", (out, in_, AX.X), {}, [in_], [out])

    def dma(self, out, in_, eng=SP, grp=None):
        self._rec(eng, "dma_start", (out, in_), {}, [in_], [out], dma=True, grp=grp)

    def emit(self, final_wait_tensors=()):
        nc = self.nc
        ops = self.ops
        last_w = {}
        readers = {}
        last_eng = {}
        dma_since = []
        bar_deps = []
        need_bar = set()
        for i, op in enumerate(ops):
            if op.bar:
                bar_deps = list(last_eng.values()) + dma_since
                dma_since = []
                need_bar = set(self.engs.keys())
                continue
            deps = set()
            for t in op.reads | op.writes:
                for j in last_w.get(t, ()):
                    deps.add(j)
            for t in op.writes:
                for j in readers.get(t, ()):
                    deps.add(j)
            if op.grp is not None:
                deps = {j for j in deps if not (ops[j].grp == op.grp)}
            if op.eng in need_bar:
                deps.update(bar_deps)
                need_bar.discard(op.eng)
            keep = []
            for j in deps:
                oj = ops[j]
                if oj.eng == op.eng and not oj.dma:
                    if op.eng == PE or not self.same_engine_sync:
                        continue
                keep.append(j)
            op.deps = keep
            for j in keep:
                ops[j].sig = True
            for t in op.reads:
                readers.setdefault(t, []).append(i)
            for t in op.writes:
                if op.grp is not None and last_w.get(t) and ops[last_w[t][0]].grp == op.grp:
                    last_w[t].append(i)
                else:
                    last_w[t] = [i]
                readers[t] = []
            if op.dma:
                dma_since.append(i)
            else:
                last_eng[op.eng] = i
        for op in ops:
            if op.dma:
                op.sig = True
        final_ops = []
        for t in final_wait_tensors:
            for j in last_w.get(t, ()):
                ops[j].sig = True
                final_ops.append(j)
        esem = {}
        ecnt = {}
        for e in (PE, ACT, DVE, POOL):
            esem[e] = nc.alloc_semaphore(name="s_" + e)
            ecnt[e] = 0
        dsem = {}
        dcnt = {}
        waited = {e: {} for e in self.engs}
        nwaits = 0
        for i, op in enumerate(ops):
            if op.bar:
                continue
            engobj = self.engs[op.eng]
            need = {}
            for j in op.deps:
                sm, v = ops[j].semval
                if need.get(sm.name, (None, -1))[1] < v:
                    need[sm.name] = (sm, v)
            for nm, (sm, v) in need.items():
                if waited[op.eng].get(nm, -1) >= v:
                    continue
                engobj.wait_ge(sm, v)
                nwaits += 1
                waited[op.eng][nm] = v
            ins = getattr(engobj, op.fn)(*op.args, **op.kw)
            if op.sig:
                if op.dma:
                    key = sorted(op.writes)[0]
                    if key not in dsem:
                        dsem[key] = nc.alloc_semaphore(name="d_" + key)
                        dcnt[key] = 0
                    dcnt[key] += 16
                    ins.then_inc(dsem[key], 16)
                    op.semval = (dsem[key], dcnt[key])
                else:
                    ecnt[op.eng] += 1
                    ins.then_inc(esem[op.eng], 1)
                    op.semval = (esem[op.eng], ecnt[op.eng])
        for j in final_ops:
            s, v = ops[j].semval
            if waited[SP].get(s.name, -1) >= v:
                continue
            nc.sync.wait_ge(s, v)
            waited[SP][s.name] = v
        print(f"[fw] ops={len(ops)} waits={nwaits} " + " ".join(f"{e}={ecnt[e]}" for e in ecnt) + f" dma_sems={len(dsem)}", flush=True)

D = 2048
HD = 128
EPS = 1e-6
NEG = -30000.0
SCALE = HD ** -0.5
NEXP = 32

CHUNKS = ([("T", 0), ("T", 2), ("N", (2, "K", 0)), ("N", (3, "K", 2)), ("V", 0), ("V", 2)]
          + [("N", (5, "K", 4 + 2 * i)) for i in range(4)] + [("V", 4 + 2 * i) for i in range(4)] + [("F", 0)]
          + [("N", (6, "Q", 2 * i)) for i in range(4)] + [("N", (7, "Q", 8 + 2 * i)) for i in range(4)])
NCH = len(CHUNKS)


def build(stop=99, nexp=NEXP, dbg_names=()):
    nc = bass.Bass("TRN2", target_bir_lowering=False)
    k = K(nc)
    used_inputs = []
    dbg_out = []

    def din(name, shape):
        used_inputs.append(name)
        return nc.dram_tensor(name, list(shape), F32, kind="ExternalInput").ap()

    def dscr(name, shape, dt=BF16):
        return nc.dram_tensor(name, list(shape), dt, kind="Internal").ap()

    class Arena:
        def __init__(self, base, size):
            self.base, self.size, self.off = base, size, 0

        def reset(self):
            self.off = 0

        def __call__(self, name, shape, dt=F32):
            nb = int(np.prod(shape[1:])) * (2 if dt == BF16 else 4)
            nb = (nb + 31) // 32 * 32
            assert self.off + nb <= self.size, (name, self.off, nb, self.size)
            t = nc.alloc_sbuf_tensor_at(name, list(shape), dt, offset=self.base + self.off)
            self.off += nb
            return t.ap()

    KB = 1024
    PA = Arena(16 * KB, 44 * KB)
    WA = Arena(60 * KB, 32 * KB)
    B1 = Arena(92 * KB, 64 * KB)
    X2A = Arena(156 * KB, 64 * KB)
    PS = [nc.alloc_psum_tensor(f"ps{i}", [128, 512], F32).ap() for i in range(8)]
    WB = [WA(f"wb{i}", [128, 16, 256], BF16) for i in range(4)]
    cnt = {"w": 0, "p": 0, "t": 0, "o": 0, "e": 0}

    def nxt(key, n):
        v = cnt[key] % n
        cnt[key] += 1
        return v

    def dbg(name, ap):
        if name in dbg_names:
            shp = list(ap.shape)
            o = nc.dram_tensor("dbg_" + name, shp, ap.dtype if hasattr(ap, "dtype") else F32, kind="ExternalOutput").ap()
            k.dma(o, ap, eng=SP)
            dbg_out.append("dbg_" + name)

    def finish():
        k.emit(final_wait_tensors=["out"] + dbg_out if stop >= 12 else dbg_out)
        return nc, used_inputs, dbg_out

    identF = PA("identF", [128, 128]); k.dma(identF, din("identF", [128, 128]))
    identB = PA("identB", [128, 128], BF16); k.copy(identB, identF)
    cT = PA("cT", [128, 16]); k.dma(cT, din("cT", [128, 16]))
    na = PA("na", [128, 16]); k.dma(na, din("na", [128, 16]))
    nm_ = PA("nm", [128, 16]); k.dma(nm_, din("nm", [128, 16]))
    cols = PA("cols", [128, 6, 16])
    A1 = PA("A1", [128, 16]); A2 = PA("A2", [128, 16])
    gAB = PA("gAB", [128, 2048]); gMB = PA("gMB", [128, 2048])
    gB = {2: gAB, 5: gMB}
    fgsb = PA("fgsb", [128, 16, 32])
    small = PA("small", [128, 64])
    ss, rs, rstd = small[:, 0:1], small[:, 1:2], small[:, 2:3]
    ssq, rq, rstdq = small[:, 4:6], small[:, 6:8], small[:, 8:10]
    rc_, fac_ = small[:, 10:12], small[:, 12:14]

    X2A.reset()
    sc_bf = X2A("sc_bf", [128, 16], BF16)
    scB = X2A("scB", [128, 16, 128], BF16)
    k.act(sc_bf, cT, AF.Silu)
    for kt in range(16):
        k.copy(scB[:, kt, :], sc_bf[:, kt:kt + 1].to_broadcast([128, 128]))
    modw = din("modw", [2048, 12288]).rearrange("(kt p) n -> p kt n", p=128)
    modbB = din("modbB", [128, 12288])
    mbs = [X2A(f"mb{i}", [128, 256]) for i in range(2)]
    tmp2 = X2A("tmp2", [128, 256]); tmp3 = X2A("tmp3", [128, 128])
    for ch in range(48):
        v, sub = ch // 8, ch % 8
        wb = WB[nxt("w", 4)]
        k.dma(wb, modw[:, :, ch * 256:(ch + 1) * 256], eng=POOL)
        mb = mbs[ch % 2]
        k.dma(mb, modbB[:, ch * 256:(ch + 1) * 256])
        ps = PS[nxt("p", 4)][:, 0:256]
        for kt in range(16):
            k.mm(ps, scB[:, kt, :], wb[:, kt, :], start=(kt == 0), stop=(kt == 15))
        if v in gB:
            k.tt(gB[v][:, sub * 256:(sub + 1) * 256], ps, mb, ALU.add)
        else:
            k.tt(tmp2, ps, mb, ALU.add)
            for jj in range(2):
                kt = sub * 2 + jj
                k.tt(tmp3, tmp2[:, jj * 128:(jj + 1) * 128], identF, ALU.mult)
                k.reduce_sum(cols[:, v, kt:kt + 1], tmp3)
    k.ts(A1, cols[:, 1, :], 1.0, None, ALU.add); k.tt(A1, A1, na, ALU.mult)
    k.ts(A2, cols[:, 4, :], 1.0, None, ALU.add); k.tt(A2, A2, nm_, ALU.mult)
    Bs1, Bs2 = cols[:, 0, :], cols[:, 3, :]
    dbg("A1", A1); dbg("gAB", gAB)
    if stop <= 1:
        return finish()

    k.barrier()
    X2A.reset(); B1.reset()
    hT = B1("hT", [128, 16, 2048], BF16)
    xts = [X2A(f"xt{i}", [128, 2048]) for i in range(2)]
    junk = X2A("junk", [128, 2048], BF16)
    xn = X2A("xn", [128, 2048], BF16)
    xloc = din("xloc", [2048, 2048])

    def norm_transpose(src, dstT, col0, Acol, Bcol):
        k.act(junk, src, AF.Square, accum_out=ss)
        k.act(rs, ss, AF.Sqrt, bias=EPS, scale=1.0 / D)
        k.recip(rstd, rs)
        k.ts(xn, src, rstd, None, ALU.mult)
        for k4 in range(4):
            pt = PS[4 + nxt("t", 2)]
            for j in range(4):
                kt = k4 * 4 + j
                k.mm(pt[:, j * 128:(j + 1) * 128], xn[:, kt * 128:(kt + 1) * 128], identB)
            for j in range(4):
                kt = k4 * 4 + j
                if j % 2 == 0:
                    k.act(dstT[:, kt, col0:col0 + 128], pt[:, j * 128:(j + 1) * 128], AF.Identity, bias=Bcol[:, kt:kt + 1], scale=Acol[:, kt:kt + 1])
                else:
                    k.ts(dstT[:, kt, col0:col0 + 128], pt[:, j * 128:(j + 1) * 128], Acol[:, kt:kt + 1], Bcol[:, kt:kt + 1], ALU.mult, ALU.add)

    for m in range(16):
        xt = xts[m % 2]
        k.dma(xt, xloc[m * 128:(m + 1) * 128, :])
        norm_transpose(xt, hT, m * 128, A1, Bs1)
    dbg("hT", hT[:, 0, :])
    if stop <= 2:
        return finish()

    k.barrier()
    X2A.reset()
    KTs = dscr("KTs", [12, 128, 2048]); KCr = dscr("KCr", [4, 128, 2048])
    Vs = dscr("Vs", [2048, 12, 128]); QTs = dscr("QTs", [16, 128, 1024])
    gt = X2A("gt", [128, 8, 128])
    gt_in = din("gt", [128, 6, 128])
    k.dma(gt[:, 0:6, :], gt_in)
    k.ts(gt[:, 6, :], gt[:, 0, :], SCALE, None, ALU.mult)
    k.ts(gt[:, 7, :], gt[:, 4, :], SCALE, None, ALU.mult)
    stgs = [X2A(f"stg{i}", [128, 256], BF16) for i in range(4)]
    st2s = [X2A(f"st2{i}", [128, 2, 128], BF16) for i in range(4)]
    junkh = X2A("junkh", [128, 128], BF16)
    winp = din("winp", [2048, NCH * 256]).rearrange("(kt p) n -> p kt n", p=128)
    pend = [None]

    def stage_c():
        if pend[0] is None:
            return
        kind, arg, m, stg = pend[0]
        pend[0] = None
        pt = PS[4 + nxt("t", 2)]
        for j in range(2):
            k.mm(pt[:, j * 128:(j + 1) * 128], stg[:, j * 128:(j + 1) * 128], identB)
        st2 = st2s[nxt("o", 4)]
        k.copy(st2, pt[:, 0:256].rearrange("p (j t) -> p j t", j=2))
        if kind == "T":
            dst = KCr[arg:arg + 2, :, m * 128:(m + 1) * 128]
        elif arg[1] == "K":
            dst = KTs[arg[2]:arg[2] + 2, :, m * 128:(m + 1) * 128]
        else:
            dst = QTs[arg[2]:arg[2] + 2, :, (m - 8) * 128:(m - 7) * 128]
        k.dma(dst.rearrange("j d t -> d j t"), st2, grp="s3")

    for ci, (kind, arg) in enumerate(CHUNKS):
        wb = WB[nxt("w", 4)]
        k.dma(wb, winp[:, :, ci * 256:(ci + 1) * 256], eng=POOL)
        tiles = range(16) if ci < 15 else range(8, 16)
        for m in tiles:
            ps = PS[nxt("p", 4)][:, 0:256]
            for kt in range(16):
                k.mm(ps, hT[:, kt, m * 128:(m + 1) * 128], wb[:, kt, :], start=(kt == 0), stop=(kt == 15))
            stage_c()
            if kind == "F":
                k.copy(fgsb[:, m, :], ps[:, 0:32], eng=ACT)
                continue
            stg = stgs[nxt("e", 4)]
            if kind == "V":
                k.copy(stg, ps, eng=ACT)
                k.dma(Vs[m * 128:(m + 1) * 128, arg:arg + 2, :], stg.rearrange("p (j d) -> p j d", j=2), grp="s3")
                continue
            if kind == "T":
                k.copy(stg, ps, eng=ACT)
            else:
                gi = arg[0]
                for j in range(2):
                    k.act(junkh, ps[:, j * 128:(j + 1) * 128], AF.Square, accum_out=ssq[:, j:j + 1])
                k.act(rq, ssq, AF.Sqrt, bias=EPS, scale=1.0 / HD)
                k.recip(rstdq, rq)
                for j in range(2):
                    k.stt(stg[:, j * 128:(j + 1) * 128], ps[:, j * 128:(j + 1) * 128], rstdq[:, j:j + 1], gt[:, gi, :], ALU.mult, ALU.mult)
            pend[0] = (kind, arg, m, stg)
    stage_c()
    if "QTs" in dbg_names:
        qd = X2A("qd", [128, 1024], BF16); k.dma(qd, QTs[0]); dbg("QTs", qd)
        kd = X2A("kd", [128, 2048], BF16); k.dma(kd, KTs[4]); dbg("KTs", kd)
    dbg("fgsb", fgsb)
    if stop <= 3:
        return finish()

    k.barrier()
    X2A.reset(); B1.reset()
    def cload(ar, name, shape, dt=F32, eng=SP, src=None):
        t = ar(name, shape, dt)
        k.dma(t, src if src is not None else din(name, shape), eng=(POOL if dt == BF16 else eng))
        return t
    triF = cload(X2A, "triF", [128, 128]); onesF = cload(X2A, "onesF", [128, 128]); e64 = cload(X2A, "e64", [128, 128])
    fbias = cload(X2A, "fbias", [128, 8]); gbias = cload(X2A, "gbias", [128, 24])
    padk8 = cload(PA, "padk8", [128, 16, 8]); farb = cload(X2A, "farb", [128, 8])
    padn = cload(PA, "padn", [128, 1])
    padfar = PA("padfar", [128, 16, 8]); biasF = PA("biasF", [128, 8, 16, 8]); gsig = PA("gsig", [128, 8, 24])
    fl = X2A("fl", [128, 16, 8]); sg_ = X2A("sg_", [128, 128]); ls = X2A("ls", [128, 128])
    wsb = X2A("wsb", [128, 128]); tot = X2A("tot", [128, 16, 8]); offs = X2A("offs", [128, 16, 8])
    cum = X2A("cum", [128, 16, 8]); crefs = X2A("crefs", [128, 16, 8]); cumpad = X2A("cumpad", [128, 16, 8])
    for m in range(16):
        k.tt(fl[:, m, :], fgsb[:, m, 0:8], fbias, ALU.add)
        k.tt(padfar[:, m, :], padk8[:, m, :], farb, ALU.add)
    flf = fl.rearrange("p m h -> p (m h)")
    k.act(sg_, flf, AF.Sigmoid); k.act(ls, sg_, AF.Ln)
    k.mm(PS[0][:, 0:128], triF, ls); k.mm(PS[1][:, 0:128], onesF, ls)
    k.copy(wsb, PS[0][:, 0:128]); k.copy(tot.rearrange("p m h -> p (m h)"), PS[1][:, 0:128])
    k.memset(offs[:, 0, :], 0.0)
    for m in range(1, 16):
        k.tt(offs[:, m, :], offs[:, m - 1, :], tot[:, m - 1, :], ALU.add)
    cumf = cum.rearrange("p m h -> p (m h)")
    k.tt(cumf, wsb, offs.rearrange("p m h -> p (m h)"), ALU.add)
    k.mm(PS[2][:, 0:128], e64, cumf)
    k.copy(crefs.rearrange("p m h -> p (m h)"), PS[2][:, 0:128])
    k.tt(cumpad, cum, padk8, ALU.subtract)
    for i in range(8):
        for kb in range(9 + i):
            k.tt(biasF[:, i, kb, :], crefs[:, 8 + i, :], cumpad[:, kb, :], ALU.subtract)
        k.tt(gsig[:, i, :], fgsb[:, 8 + i, 8:32], gbias, ALU.add)
    k.act(gsig.rearrange("p i g -> p (i g)"), gsig.rearrange("p i g -> p (i g)"), AF.Sigmoid)
    dbg("biasF", biasF.rearrange("p i k h -> p (i k h)")); dbg("gsig", gsig.rearrange("p i g -> p (i g)"))
    if stop <= 4:
        return finish()

    k.barrier()
    X2A.reset()
    oacc = X2A("oacc", [128, 8, 1024])
    B1.reset()
    AT = Arena(B1.base, 32 * KB)
    mixT = Arena(B1.base + 32 * KB, 32 * KB)("mixT", [128, 16, 1024], BF16)
    XT = Arena(X2A.base + 32 * KB, 32 * KB)
    gtc = XT("gtc", [128, 6, 128]); k.dma(gtc, gt_in)
    KCT = [PA(f"KCT{g}", [128, 127], BF16) for g in range(2)]
    VCa = [PA(f"VCa{g}", [128, 161], BF16) for g in range(2)]
    ovl_in = din("ovl", [127, 32])
    for g in range(2):
        k.memset(VCa[g][:, 128:129], 1.0)
        k.dma(VCa[g][0:127, 129:161], ovl_in, eng=POOL)
    posT = cload(XT, "posT", [128, 2, 32])
    w1_in = din("w1", [2, 4096, 256]); w2_in = din("w2", [2, 256, 128])
    w1sb = AT("w1sb", [128, 32, 256], BF16); w2sb = AT("w2sb", [128, 2, 128], BF16)
    kcraw = AT("kcraw", [128, 2048], BF16); tmpl = AT("tmpl", [128, 32, 127], BF16)
    GT = [AT(f"GT{i}", [128, 127], BF16) for i in range(2)]
    gx2 = XT("gx2", [128, 127]); gu = XT("gu", [128, 127]); gth = XT("gth", [128, 127])
    kcn = XT("kcn", [128, 128], BF16); junkc = XT("junkc", [128, 128], BF16)
    for kv in range(2):
        k.dma(w1sb, w1_in[kv].rearrange("(l d) h -> d l h", d=128), eng=POOL)
        k.dma(w2sb, w2_in[kv].rearrange("(hh p) d -> p hh d", p=128), eng=POOL)
        for g in range(2):
            k.dma(kcraw, KCr[kv * 2 + g])
            for l in range(32):
                k.ts(tmpl[:, l, :], kcraw[:, l:l + 2017:16], posT[:, kv, l:l + 1], None, ALU.add)
            for hh in range(2):
                ps = PS[hh][:, 0:127]
                for l in range(32):
                    k.mm(ps, w1sb[:, l, hh * 128:(hh + 1) * 128], tmpl[:, l, :], start=(l == 0), stop=(l == 31))
                k.act(gx2, ps, AF.Square)
                k.ts(gu, gx2, 0.044715, 1.0, ALU.mult, ALU.add)
                k.tt(gu, gu, ps, ALU.mult)
                k.act(gth, gu, AF.Tanh, scale=0.7978845608028654)
                k.ts(gth, gth, 0.5, 0.5, ALU.mult, ALU.add)
                k.tt(GT[hh], gth, ps, ALU.mult)
            pk = PS[2][0:127, 0:128]
            k.mm(pk, GT[0], w2sb[:, 0, :], start=True, stop=False)
            k.mm(pk, GT[1], w2sb[:, 1, :], start=False, stop=True)
            if kv == 0:
                k.act(junkc[0:127, :], pk, AF.Square, accum_out=ss[0:127, :])
                k.act(rs[0:127, :], ss[0:127, :], AF.Sqrt, bias=EPS, scale=1.0 / HD)
                k.recip(rstd[0:127, :], rs[0:127, :])
                k.stt(kcn[0:127, :], pk, rstd[0:127, :], gtc[0:127, 1, :], ALU.mult, ALU.mult)
                k.mm(PS[4][:, 0:127], kcn[0:127, :], identB[0:127, 0:127])
                k.copy(KCT[g], PS[4][:, 0:127])
            else:
                k.copy(VCa[g][0:127, 0:128], pk)
    dbg("KCT0", KCT[0]); dbg("VCa0", VCa[0])
    if stop <= 5:
        return finish()

    k.barrier()
    AT.reset(); XT.reset()
    qTs = [AT(f"qT{i}", [128, 1024], BF16) for i in range(2)]
    bcs = [AT(f"bc{i}", [128, 1024], BF16) for i in range(2)]
    pTs = [AT(f"pT{i}", [128, 512], BF16) for i in range(4)]
    vAs = [AT(f"vA{i}", [128, 16, 129], BF16) for i in range(2)]
    for v_ in vAs:
        k.memset(v_[:, :, 128:129], 1.0)
    negselT = [AT(f"negsel{g}", [32, 1024], BF16) for g in range(2)]
    t01 = cload(AT, "t01", [128, 8, 2, 128], BF16)
    wm4 = cload(AT, "wm4", [128, 128], BF16); causalB = cload(AT, "causal", [128, 128], BF16)
    imp = XT("imp", [128, 8, 32]); vmul = cload(XT, "vmul", [128, 8, 32]); amask = cload(XT, "amask", [128, 8, 32])
    impm = XT("impm", [128, 32]); wk_ = XT("wk_", [128, 32]); m8 = XT("m8", [128, 16]); nsel = XT("nsel", [128, 32], BF16)
    onormB = cload(XT, "onormB", [128, 2048])
    kTs = [XT(f"kT{i}", [128, 2048], BF16) for i in range(2)]
    ekb = cload(XT, "ekb", [32, 16, 128], BF16)
    mix = XT("mix", [128, 1024], BF16); junko = XT("junko", [128, 1024], BF16)
    biascmp_in = din("biascmp", [8, 127, 1024])
    qcnt = [0]

    def load_q(hq):
        q = qTs[qcnt[0] % 2]; qcnt[0] += 1
        k.dma(q, QTs[hq])
        return q

    for g in range(2):
        for r in range(4):
            h = 4 * g + r
            qT = load_q(h)
            bc = bcs[h % 2]
            k.dma(bc[0:127, :], biascmp_in[h], eng=POOL)
            for half in range(2):
                ps = PS[nxt("p", 4)][0:127, :]
                k.mm(ps, KCT[g], qT[:, half * 512:(half + 1) * 512], start=True, stop=False)
                k.mm(ps, identB[0:127, 0:127], bc[0:127, half * 512:(half + 1) * 512], start=False, stop=True)
                pT = pTs[nxt("e", 4)]
                k.act(pT[0:127, :], ps, AF.Exp, bias=padn[0:127, :])
                for i4 in range(4):
                    i = half * 4 + i4
                    po = PS[6 + nxt("o", 2)][:, 0:161]
                    k.mm(po, pT[0:127, i4 * 128:(i4 + 1) * 128], VCa[g][0:127, :])
                    k.ts(rc_[:, 1:2], po[:, 128:129], 1e-30, None, ALU.max)
                    k.recip(rc_[:, 0:1], rc_[:, 1:2])
                    k.tt(fac_[:, 0:1], rc_[:, 0:1], gsig[:, i, h:h + 1], ALU.mult)
                    k.ts(oacc[:, i, h * 128:(h + 1) * 128], po[:, 0:128], fac_[:, 0:1], None, ALU.mult)
                    if r == 0:
                        k.ts(imp[:, i, :], po[:, 129:161], rc_[:, 0:1], None, ALU.mult)
                    else:
                        k.stt(imp[:, i, :], po[:, 129:161], rc_[:, 0:1], imp[:, i, :], ALU.mult, ALU.add)
        for i in range(8):
            k.tt(impm, imp[:, i, :], vmul[:, i, :], ALU.mult)
            k.tt(impm, impm, amask[:, i, :], ALU.add)
            k.max8(m8[:, 0:8], impm)
            k.match_replace(wk_, m8[:, 0:8], impm, -1e9)
            k.max8(m8[:, 8:16], wk_)
            k.ts(nsel, impm, m8[:, 15:16], -NEG, ALU.is_ge, ALU.mult)
            k.ts(nsel, nsel, NEG, None, ALU.add)
            k.mm(PS[4][0:32, 0:128], nsel, identB)
            k.copy(negselT[g][:, i * 128:(i + 1) * 128], PS[4][0:32, 0:128])
    dbg("oacc_cmp", oacc[:, 0, :]); dbg("negsel0", negselT[0])
    if stop <= 6:
        return finish()

    kvc = [0]

    def load_kv(idx):
        kT = kTs[kvc[0] % 2]; vA = vAs[kvc[0] % 2]; kvc[0] += 1
        k.dma(kT, KTs[idx])
        k.dma(vA[:, :, 0:128], Vs[:, idx, :].rearrange("(m p) d -> p m d", p=128))
        return kT, vA

    def attend(kT, vA, qT, h, i, kbs, mode, g):
        qb = 8 + i
        po = PS[6 + nxt("o", 2)][:, 0:129]

        def qk(kb):
            dl = qb - kb
            ps = PS[nxt("p", 4)][:, 0:128]
            extra = []
            if mode == "fox":
                if dl == 0:
                    extra.append((identB, causalB))
                bias = biasF[:, i, kb, h:h + 1]
            else:
                if dl <= 1:
                    extra.append((identB, t01[:, h, dl, :]))
                    bias = padk8[:, kb, 0:1]
                else:
                    bias = padfar[:, kb, h:h + 1]
                    if mode == "win" and dl == 4:
                        extra.append((identB, wm4))
                if mode == "slc":
                    extra.append((ekb[:, kb, :], negselT[g][:, i * 128:(i + 1) * 128]))
            k.mm(ps, kT[:, kb * 128:(kb + 1) * 128], qT[:, i * 128:(i + 1) * 128], start=True, stop=(len(extra) == 0))
            for xi, (l_, r_) in enumerate(extra):
                k.mm(ps, l_, r_, start=False, stop=(xi == len(extra) - 1))
            return ps, bias

        pend = [qk(kb) for kb in kbs[:2]]
        for idx, kb in enumerate(kbs):
            if idx + 2 < len(kbs):
                pend.append(qk(kbs[idx + 2]))
            ps, bias = pend.pop(0)
            pT = pTs[nxt("e", 4)][:, 0:128]
            k.act(pT, ps, AF.Exp, bias=bias)
            k.mm(po, pT, vA[:, kb, :], start=(idx == 0), stop=(idx == len(kbs) - 1))
        return po

    for g in range(2):
        for mode, kidx, goff in (("slc", g, 8), ("win", 2 + g, 16)):
            kT, vA = load_kv(kidx)
            for r in range(4):
                h = 4 * g + r
                qT = load_q(h)
                for i in range(8):
                    qb = 8 + i
                    kbs = list(range(qb + 1)) if mode == "slc" else list(range(qb - 4, qb + 1))
                    po = attend(kT, vA, qT, h, i, kbs, mode, g)
                    k.recip(rc_[:, 0:1], po[:, 128:129])
                    k.tt(fac_[:, 0:1], rc_[:, 0:1], gsig[:, i, goff + h:goff + h + 1], ALU.mult)
                    k.stt(oacc[:, i, h * 128:(h + 1) * 128], po[:, 0:128], fac_[:, 0:1], oacc[:, i, h * 128:(h + 1) * 128], ALU.mult, ALU.add)

    def out_norm(koff):
        for i in range(8):
            k.act(junko, oacc[:, i, :], AF.Square, accum_out=ss)
            k.act(rs, ss, AF.Sqrt, bias=EPS, scale=1.0 / 1024)
            k.recip(rstd, rs)
            k.stt(mix, oacc[:, i, :], rstd, onormB[:, koff * 128:koff * 128 + 1024], ALU.mult, ALU.mult)
            for k4 in range(2):
                pt = PS[4 + nxt("t", 2)]
                for j in range(4):
                    kt = k4 * 4 + j
                    k.mm(pt[:, j * 128:(j + 1) * 128], mix[:, kt * 128:(kt + 1) * 128], identB)
                k.copy(mixT[:, koff + k4 * 4:koff + k4 * 4 + 4, i * 128:(i + 1) * 128], pt.rearrange("p (j t) -> p j t", j=4), eng=(ACT if k4 == 0 else DVE))

    dbg("oacc_nsa", oacc[:, 0, :])
    out_norm(0)
    if stop <= 7:
        dbg("mixT", mixT[:, 0, :])
        return finish()
    for h in range(8):
        kT, vA = load_kv(4 + h)
        qT = load_q(8 + h)
        for i in range(8):
            po = attend(kT, vA, qT, h, i, list(range(9 + i)), "fox", 0)
            k.recip(rc_[:, 0:1], po[:, 128:129])
            k.ts(oacc[:, i, h * 128:(h + 1) * 128], po[:, 0:128], rc_[:, 0:1], None, ALU.mult)
    dbg("oacc_fox", oacc[:, 0, :])
    out_norm(8)
    dbg("mixT", mixT[:, 8, :])
    if stop <= 8:
        return finish()

    k.barrier()
    X2A.reset(); AT.reset()
    x2 = X2A("x2", [128, 8, 2048])
    for i in range(8):
        k.dma(x2[:, i, :], xloc[(8 + i) * 128:(9 + i) * 128, :])
    tmps = [AT(f"tmpo{i}", [128, 512]) for i in range(3)]
    wout = din("wout", [2048, 2048]).rearrange("(kt p) n -> p kt n", p=128)
    for c in range(8):
        wb = WB[nxt("w", 4)]
        k.dma(wb, wout[:, :, c * 256:(c + 1) * 256], eng=POOL)
        for i in range(8):
            ps = PS[nxt("p", 4)][:, 0:256]
            for kt in range(16):
                k.mm(ps, mixT[:, kt, i * 128:(i + 1) * 128], wb[:, kt, :], start=(kt == 0), stop=(kt == 15))
            tmp = tmps[nxt("e", 3)][:, 0:256]
            k.tt(tmp, ps, gAB[:, c * 256:(c + 1) * 256], ALU.mult)
            k.tt(x2[:, i, c * 256:(c + 1) * 256], x2[:, i, c * 256:(c + 1) * 256], tmp, ALU.add)
    dbg("x2", x2[:, 0, :])
    if stop <= 9:
        return finish()

    k.barrier()
    B1.reset()
    h2T = B1("h2T", [128, 16, 1024], BF16)
    actT = B1("actT", [128, 16, 1024], BF16)
    MT = Arena(B1.base + 32 * KB, 32 * KB)
    junk2 = MT("junk2", [128, 2048], BF16); xn2 = MT("xn2", [128, 2048], BF16)
    junk, xn = junk2, xn2
    def norm_transpose2(src, dstT, col0, Acol, Bcol):
        k.act(junk2, src, AF.Square, accum_out=ss)
        k.act(rs, ss, AF.Sqrt, bias=EPS, scale=1.0 / D)
        k.recip(rstd, rs)
        k.ts(xn2, src, rstd, None, ALU.mult)
        for k4 in range(4):
            pt = PS[4 + nxt("t", 2)]
            for j in range(4):
                kt = k4 * 4 + j
                k.mm(pt[:, j * 128:(j + 1) * 128], xn2[:, kt * 128:(kt + 1) * 128], identB)
            for j in range(4):
                kt = k4 * 4 + j
                if j % 2 == 0:
                    k.act(dstT[:, kt, col0:col0 + 128], pt[:, j * 128:(j + 1) * 128], AF.Identity, bias=Bcol[:, kt:kt + 1], scale=Acol[:, kt:kt + 1])
                else:
                    k.ts(dstT[:, kt, col0:col0 + 128], pt[:, j * 128:(j + 1) * 128], Acol[:, kt:kt + 1], Bcol[:, kt:kt + 1], ALU.mult, ALU.add)
    for i in range(8):
        norm_transpose2(x2[:, i, :], h2T, i * 128, A2, Bs2)
    rwsb = MT("rwsb", [128, 16, 32], BF16)
    k.dma(rwsb, din("rw", [2048, 32]).rearrange("(kt p) e -> p kt e", p=128), eng=POOL)
    rbB = cload(MT, "rbB", [128, 32])
    gates = PA("gates", [128, 8, 32]); bupT = cload(PA, "bupT", [128, 32, 32])
    lg = MT("lg", [128, 32]); m8r = MT("m8r", [128, 8]); nmx = MT("nmx", [128, 1]); msk = MT("msk", [128, 32])
    ex = MT("ex", [128, 32]); rsm = MT("rsm", [128, 1]); rrm = MT("rrm", [128, 1])
    gTs = MT("gTs", [32, 128]); bdn = cload(MT, "bdn", [32, 2048])
    tmpb = [MT(f"tmpb{i}", [128, 512]) for i in range(2)]
    for i in range(8):
        ps = PS[nxt("p", 4)][:, 0:32]
        for kt in range(16):
            k.mm(ps, h2T[:, kt, i * 128:(i + 1) * 128], rwsb[:, kt, :], start=(kt == 0), stop=(kt == 15))
        k.tt(lg, ps, rbB, ALU.add)
        k.max8(m8r, lg)
        k.ts(nmx, m8r[:, 0:1], -1.0, None, ALU.mult)
        k.ts(msk, lg, m8r[:, 3:4], None, ALU.is_ge)
        k.act(ex, lg, AF.Exp, bias=nmx)
        k.tt(ex, ex, msk, ALU.mult)
        k.reduce_sum(rsm, ex)
        k.recip(rrm, rsm)
        k.ts(gates[:, i, :], ex, rrm, None, ALU.mult)
        k.mm(PS[4][0:32, 0:128], gates[:, i, :], identF)
        k.copy(gTs, PS[4][0:32, 0:128])
        for c in range(4):
            pb = PS[nxt("p", 4)]
            k.mm(pb, gTs, bdn[:, c * 512:(c + 1) * 512])
            tb = tmpb[c % 2]
            k.tt(tb, pb, gMB[:, c * 512:(c + 1) * 512], ALU.mult)
            k.tt(x2[:, i, c * 512:(c + 1) * 512], x2[:, i, c * 512:(c + 1) * 512], tb, ALU.add)
    dbg("gates", gates.rearrange("p i e -> p (i e)")); dbg("h2T", h2T[:, 0, :])
    if stop <= 10:
        return finish()

    k.barrier()
    ET = Arena(PA.base + PA.off, PA.size - PA.off)
    tg = ET("tg", [128, 512]); tsg = ET("tsg", [128, 512]); tl = ET("tl", [128, 512]); ta = ET("ta", [128, 512])
    tds = [ET(f"td{i}", [128, 256]) for i in range(2)]
    wup = din("wup", [NEXP, 2048, 4096]); wdn = din("wdn", [NEXP, 2048, 2048])
    pset = [0]
    for e in range(nexp):
        wu = wup[e].rearrange("(kt p) f -> p kt f", p=128)
        for c in range(8):
            wg = WB[nxt("w", 4)]; k.dma(wg, wu[:, :, c * 256:(c + 1) * 256], eng=POOL)
            wl = WB[nxt("w", 4)]; k.dma(wl, wu[:, :, 2048 + c * 256:2048 + (c + 1) * 256], eng=POOL)
            for j in range(2):
                ft = c * 2 + j
                base = 4 * (pset[0] % 2); pset[0] += 1
                for kt in range(16):
                    for tc in range(2):
                        k.mm(PS[base + tc], wg[:, kt, j * 128:(j + 1) * 128], h2T[:, kt, tc * 512:(tc + 1) * 512], start=(kt == 0), stop=(kt == 15))
                for kt in range(16):
                    for tc in range(2):
                        k.mm(PS[base + 2 + tc], wl[:, kt, j * 128:(j + 1) * 128], h2T[:, kt, tc * 512:(tc + 1) * 512], start=(kt == 0), stop=(kt == 15))
                for tc in range(2):
                    k.ts(tg, PS[base + tc], bupT[:, e, ft:ft + 1], 7.0, ALU.add, ALU.min)
                    k.act(tsg, tg, AF.Sigmoid, scale=1.702)
                    k.ts(tl, PS[base + 2 + tc], bupT[:, e, 16 + ft:17 + ft], 7.0, ALU.add, ALU.min)
                    k.ts(tl, tl, -7.0, 1.0, ALU.max, ALU.add)
                    k.tt(ta, tg, tsg, ALU.mult)
                    k.tt(actT[:, ft, tc * 512:(tc + 1) * 512], ta, tl, ALU.mult)
        wd_ = wdn[e].rearrange("(kt p) d -> p kt d", p=128)
        for c in range(8):
            wd = WB[nxt("w", 4)]; k.dma(wd, wd_[:, :, c * 256:(c + 1) * 256], eng=POOL)
            for i in range(8):
                ps = PS[nxt("p", 8)][:, 0:256]
                for kt in range(16):
                    k.mm(ps, actT[:, kt, i * 128:(i + 1) * 128], wd[:, kt, :], start=(kt == 0), stop=(kt == 15))
                td = tds[nxt("e", 2)]
                k.tt(td, ps, gMB[:, c * 256:(c + 1) * 256], ALU.mult)
                k.stt(x2[:, i, c * 256:(c + 1) * 256], td, gates[:, i, e:e + 1], x2[:, i, c * 256:(c + 1) * 256], ALU.mult, ALU.add)

    outd = nc.dram_tensor("out", [1024, 2048], F32, kind="ExternalOutput").ap()
    for i in range(8):
        k.dma(outd[i * 128:(i + 1) * 128, :], x2[:, i, :])
    stop = 12
    return finish()


def _t5_bucket(dist):
    n = np.maximum(dist, 0)
    nf = np.maximum(n, 1).astype(np.float32)
    large = 16 + (np.log(nf / np.float32(16)) / np.float32(math.log(8.0)) * np.float32(16)).astype(np.int32)
    large = np.minimum(large, 31)
    return np.where(n < 16, n, large)


def host_prep(inp, core):
    f32 = np.float32
    b, p = core // 2, core % 2
    x = np.asarray(inp["x"], f32)
    o = {}
    if p == 1:
        o["xloc"] = np.ascontiguousarray(x[b])
    else:
        o["xloc"] = np.concatenate([np.zeros((1024, D), f32), x[b, :1024]], axis=0)
    col = lambda v: np.ascontiguousarray(np.asarray(v, f32).reshape(16, 128).T)
    rep = lambda v: np.ascontiguousarray(np.broadcast_to(np.asarray(v, f32).reshape(1, -1), (128, np.asarray(v).size)))
    o["cT"] = col(inp["c"][b])
    o["na"] = col(inp["norm_attn"][0]); o["nm"] = col(inp["norm_moe"][0])
    o["modw"] = np.asarray(inp["mod_w"][0], f32)
    o["modbB"] = rep(inp["mod_b"][0])
    o["identF"] = np.eye(128, dtype=f32)
    w = np.asarray(inp["w_in"][0], f32)
    sizes = [1024, 256, 256, 256, 256, 256, 256, 24, 1024, 1024, 1024, 8]
    offs = np.concatenate([[0], np.cumsum(sizes)])
    seg = lambda i: w[:, offs[i]:offs[i + 1]]
    fcols = np.zeros((D, 256), f32)
    fcols[:, 0:8] = seg(11); fcols[:, 8:32] = seg(7)
    o["winp"] = np.ascontiguousarray(np.concatenate(
        [seg(1), seg(2), seg(3), seg(5), seg(4), seg(6), seg(9), seg(10), fcols, seg(0), seg(8)], axis=1))
    kn = np.asarray(inp["nsa_k_norm"][0], f32)
    gts = [inp["nsa_q_norm"][0], kn[0], kn[1], kn[2], inp["fox_q_norm"][0], inp["fox_k_norm"][0]]
    o["gt"] = np.ascontiguousarray(np.stack([rep(g) for g in gts], axis=1))
    o["fbias"] = rep(inp["fox_forget_bias"][0]); o["gbias"] = rep(inp["b_nsa_gate"][0])
    o["posT"] = np.ascontiguousarray(np.stack([np.asarray(inp["cmp_pos_k"][0], f32).T, np.asarray(inp["cmp_pos_v"][0], f32).T], axis=1))
    o["w1"] = np.ascontiguousarray(np.stack([inp["cmp_k_w1"][0], inp["cmp_v_w1"][0]]).astype(f32))
    o["w2"] = np.ascontiguousarray(np.stack([inp["cmp_k_w2"][0], inp["cmp_v_w2"][0]]).astype(f32))
    n = np.arange(127)[:, None]; j = np.arange(32)[None, :]
    o["ovl"] = ((16 * n < 64 * j + 64) & (16 * n + 32 > 64 * j)).astype(f32)
    rb = np.asarray(inp["rel_bias"], f32)
    t = np.arange(1024)[None, :]
    d = (1024 + t) - (16 * n + 31)
    bc = rb[_t5_bucket(d)]
    bc = np.where((d >= 0)[..., None], bc, f32(NEG))
    o["biascmp"] = np.ascontiguousarray(bc.transpose(2, 0, 1).astype(f32))
    s = np.arange(128)[:, None]; tt_ = np.arange(128)[None, :]
    t01 = np.zeros((128, 8, 2, 128), f32)
    for dl in range(2):
        dd = dl * 128 + tt_ - s
        v = rb[_t5_bucket(dd)]
        v = np.where((dd >= 0)[..., None], v, f32(NEG))
        t01[:, :, dl, :] = v.transpose(0, 2, 1)
    o["t01"] = t01
    o["farb"] = rep(rb[31])
    o["wm4"] = np.where(s > tt_, f32(0), f32(NEG)).astype(f32)
    o["causal"] = np.where(s <= tt_, f32(0), f32(NEG)).astype(f32)
    sl = np.arange(128)[:, None] + 128 * np.arange(16)[None, :]
    padk = np.where((sl < 1024) & (p == 0), f32(NEG), f32(0)).astype(f32)
    o["padk8"] = np.ascontiguousarray(np.broadcast_to(padk[:, :, None], (128, 16, 8)))
    pn = np.zeros((128, 1), f32)
    if p == 0:
        pn[:64] = NEG
    o["padn"] = pn
    tl = (1024 + np.arange(1024)).reshape(8, 128).T
    treal = tl - 1024 * (1 - p)
    cur = (treal // 64)[:, :, None]
    jr = (np.arange(32) - 16 * (1 - p))[None, None, :]
    valid = (jr >= 0) & (jr <= cur)
    f0 = valid & (jr == 0); f1 = valid & (jr == cur) & ~f0; f2 = valid & (jr == cur - 1) & ~f0 & ~f1
    am = np.where(f0, 3e6, np.where(f1, 2e6, np.where(f2, 1e6, np.where(valid, 0.0, -1e6))))
    o["amask"] = am.astype(f32)
    o["vmul"] = (valid & ~f0 & ~f1 & ~f2).astype(f32)
    ek = np.zeros((32, 16, 128), f32)
    for kb in range(16):
        ek[2 * kb, kb, :64] = 1; ek[2 * kb + 1, kb, 64:] = 1
    o["ekb"] = ek
    o["triF"] = (np.arange(128)[:, None] <= np.arange(128)[None, :]).astype(f32)
    o["onesF"] = np.ones((128, 128), f32)
    e64 = np.zeros((128, 128), f32); e64[64, :] = 1
    o["e64"] = e64
    o["onormB"] = rep(inp["out_norm"][0])
    o["wout"] = np.asarray(inp["w_out"][0], f32)
    o["rw"] = np.asarray(inp["router_w"][0], f32)
    o["rbB"] = rep(inp["router_b"][0])
    o["wup"] = np.asarray(inp["exp_w_up"][0], f32)
    o["wdn"] = np.asarray(inp["exp_w_down"][0], f32)
    bu = np.asarray(inp["exp_b_up"][0], f32)
    o["bupT"] = np.ascontiguousarray(bu.reshape(32, 32, 128).transpose(2, 0, 1))
    o["bdn"] = np.asarray(inp["exp_b_down"][0], f32)
    return o


_CACHE = {}


def kernel(**inputs):
    if "prog" not in _CACHE:
        _CACHE["prog"] = build()
    nc, used, _ = _CACHE["prog"]
    in_maps = []
    for core in range(8):
        hp = host_prep(inputs, core)
        in_maps.append({n: hp[n] for n in used})
    res = run_bass_kernel_spmd(nc, in_maps, core_ids=list(range(8)))
    out = np.zeros((4, 2048, D), np.float32)
    for core in range(8):
        b, p = core // 2, core % 2
        out[b, p * 1024:(p + 1) * 1024] = res.results[core]["out"]
    return out
```

```python
import math
import numpy as np
import concourse.bass as bass
import concourse.mybir as mybir
from concourse.bass_utils import run_bass_kernel_spmd

F32 = mybir.dt.float32
BF16 = mybir.dt.bfloat16
ALU = mybir.AluOpType
AF = mybir.ActivationFunctionType
AX = mybir.AxisListType
PE, ACT, DVE, POOL, SP = "pe", "act", "dve", "pool", "sp"


def _tname(ap):
    t = getattr(ap, "tensor", None)
    if t is None:
        t = ap
    return t.name


class Op:
    __slots__ = ("eng", "fn", "args", "kw", "reads", "writes", "dma", "deps", "sig", "semval", "grp", "bar")

    def __init__(self, eng, fn, args, kw, reads, writes, dma=False, grp=None):
        self.eng = eng
        self.fn = fn
        self.args = args
        self.kw = kw
        self.reads = reads
        self.writes = writes
        self.dma = dma
        self.deps = []
        self.sig = False
        self.semval = None
        self.grp = grp
        self.bar = False


class K:
    def __init__(self, nc, same_engine_sync=True):
        self.nc = nc
        self.ops = []
        self.same_engine_sync = same_engine_sync
        self.engs = {PE: nc.tensor, ACT: nc.scalar, DVE: nc.vector, POOL: nc.gpsimd, SP: nc.sync}

    def _rec(self, eng, fn, args, kw, reads, writes, dma=False, grp=None):
        r = set(_tname(a) for a in reads if a is not None and not isinstance(a, (int, float)))
        w = set(_tname(a) for a in writes)
        self.ops.append(Op(eng, fn, args, kw, r, w, dma, grp))

    def barrier(self):
        o = Op(None, None, None, None, set(), set())
        o.bar = True
        self.ops.append(o)

    def mm(self, out, lhsT, rhs, start=True, stop=True):
        self._rec(PE, "matmul", (out, lhsT, rhs), dict(start=start, stop=stop), [lhsT, rhs] + ([] if start else [out]), [out])

    def act(self, out, in_, func, bias=None, scale=None, accum_out=None):
        kw = {}
        reads = [in_]
        if bias is not None:
            kw["bias"] = bias
            reads.append(bias)
        if scale is not None:
            kw["scale"] = scale
            reads.append(scale)
        writes = [out]
        if accum_out is not None:
            kw["accum_out"] = accum_out
            writes.append(accum_out)
        self._rec(ACT, "activation", (out, in_, func), kw, reads, writes)

    def tt(self, out, in0, in1, op, eng=DVE):
        self._rec(eng, "tensor_tensor", (out, in0, in1, op), {}, [in0, in1], [out])

    def ts(self, out, in0, s1, s2, op0, op1=None, eng=DVE):
        kw = {}
        if op1 is not None:
            kw["op1"] = op1
        self._rec(eng, "tensor_scalar", (out, in0, s1, s2, op0), kw, [in0, s1, s2], [out])

    def stt(self, out, in0, scalar, in1, op0, op1):
        self._rec(DVE, "scalar_tensor_tensor", (out, in0, scalar, in1, op0, op1), {}, [in0, scalar, in1], [out])

    def copy(self, out, in_, eng=DVE):
        if eng == ACT:
            self._rec(ACT, "copy", (out, in_), {}, [in_], [out])
        else:
            self._rec(eng, "tensor_copy", (out, in_), {}, [in_], [out])

    def memset(self, out, val, eng=DVE):
        self._rec(eng, "memset", (out, val), {}, [], [out])

    def recip(self, out, in_):
        self._rec(DVE, "reciprocal", (out, in_), {}, [in_], [out])

    def max8(self, out, in_):
        self._rec(DVE, "max", (out, in_), {}, [in_], [out])

    def match_replace(self, out, in_to_replace, in_values, imm):
        self._rec(DVE, "match_replace", (out, in_to_replace, in_values, imm), {}, [in_to_replace, in_values], [out])

    def reduce_sum(self, out, in_, eng=DVE):
        self._rec(eng, "reduce_sum", (out, in_, AX.X), {}, [in_], [out])

    def dma(self, out, in_, eng=SP, grp=None):
        self._rec(eng, "dma_start", (out, in_), {}, [in_], [out], dma=True, grp=grp)

    def emit(self, final_wait_tensors=()):
        nc = self.nc
        ops = self.ops
        last_w = {}
        readers = {}
        last_eng = {}
        dma_since = []
        bar_deps = []
        need_bar = set()
        for i, op in enumerate(ops):
            if op.bar:
                bar_deps = list(last_eng.values()) + dma_since
                dma_since = []
                need_bar = set(self.engs.keys())
                continue
            deps = set()
            for t in op.reads | op.writes:
                for j in last_w.get(t, ()):
                    deps.add(j)
            for t in op.writes:
                for j in readers.get(t, ()):
                    deps.add(j)
            if op.grp is not None:
                deps = {j for j in deps if not (ops[j].grp == op.grp)}
            if op.eng in need_bar:
                deps.update(bar_deps)
                need_bar.discard(op.eng)
            keep = []
            for j in deps:
                oj = ops[j]
                if oj.eng == op.eng and not oj.dma:
                    if op.eng == PE or not self.same_engine_sync:
                        continue
                keep.append(j)
            op.deps = keep
            for j in keep:
                ops[j].sig = True
            for t in op.reads:
                readers.setdefault(t, []).append(i)
            for t in op.writes:
                if op.grp is not None and last_w.get(t) and ops[last_w[t][0]].grp == op.grp:
                    last_w[t].append(i)
                else:
                    last_w[t] = [i]
                readers[t] = []
            if op.dma:
                dma_since.append(i)
            else:
                last_eng[op.eng] = i
        for op in ops:
            if op.dma:
                op.sig = True
        final_ops = []
        for t in final_wait_tensors:
            for j in last_w.get(t, ()):
                ops[j].sig = True
                final_ops.append(j)
        esem = {}
        ecnt = {}
        for e in (PE, ACT, DVE, POOL):
            esem[e] = nc.alloc_semaphore(name="s_" + e)
            ecnt[e] = 0
        dsem = {}
        dcnt = {}
        waited = {e: {} for e in self.engs}
        nwaits = 0
        for i, op in enumerate(ops):
            if op.bar:
                continue
            engobj = self.engs[op.eng]
            need = {}
            for j in op.deps:
                sm, v = ops[j].semval
                if need.get(sm.name, (None, -1))[1] < v:
                    need[sm.name] = (sm, v)
            for nm, (sm, v) in need.items():
                if waited[op.eng].get(nm, -1) >= v:
                    continue
                engobj.wait_ge(sm, v)
                nwaits += 1
                waited[op.eng][nm] = v
            ins = getattr(engobj, op.fn)(*op.args, **op.kw)
            if op.sig:
                if op.dma:
                    key = sorted(op.writes)[0]
                    if key not in dsem:
                        dsem[key] = nc.alloc_semaphore(name="d_" + key)
                        dcnt[key] = 0
                    dcnt[key] += 16
                    ins.then_inc(dsem[key], 16)
                    op.semval = (dsem[key], dcnt[key])
                else:
                    ecnt[op.eng] += 1
                    ins.then_inc(esem[op.eng], 1)
                    op.semval = (esem[op.eng], ecnt[op.eng])
        for j in final_ops:
            s, v = ops[j].semval
            if waited[SP].get(s.name, -1) >= v:
                continue
            nc.sync.wait_ge(s, v)
            waited[SP][s.name] = v
        print(f"[fw] ops={len(ops)} waits={nwaits} " + " ".join(f"{e}={ecnt[e]}" for e in ecnt) + f" dma_sems={len(dsem)}", flush=True)

D = 2048
HD = 128
EPS = 1e-6
NEG = -30000.0
SCALE = HD ** -0.5
NEXP = 32

CHUNKS = ([("T", 0), ("T", 2), ("N", (2, "K", 0)), ("N", (3, "K", 2)), ("V", 0), ("V", 2)]
          + [("N", (5, "K", 4 + 2 * i)) for i in range(4)] + [("V", 4 + 2 * i) for i in range(4)] + [("F", 0)]
          + [("N", (6, "Q", 2 * i)) for i in range(4)] + [("N", (7, "Q", 8 + 2 * i)) for i in range(4)])
NCH = len(CHUNKS)


def build(stop=99, nexp=NEXP, dbg_names=()):
    nc = bass.Bass("TRN2", target_bir_lowering=False)
    k = K(nc)
    used_inputs = []
    dbg_out = []

    def din(name, shape):
        used_inputs.append(name)
        return nc.dram_tensor(name, list(shape), F32, kind="ExternalInput").ap()

    def dscr(name, shape, dt=BF16):
        return nc.dram_tensor(name, list(shape), dt, kind="Internal").ap()

    class Arena:
        def __init__(self, base, size):
            self.base, self.size, self.off = base, size, 0

        def reset(self):
            self.off = 0

        def __call__(self, name, shape, dt=F32):
            nb = int(np.prod(shape[1:])) * (2 if dt == BF16 else 4)
            nb = (nb + 31) // 32 * 32
            assert self.off + nb <= self.size, (name, self.off, nb, self.size)
            t = nc.alloc_sbuf_tensor_at(name, list(shape), dt, offset=self.base + self.off)
            self.off += nb
            return t.ap()

    KB = 1024
    PA = Arena(16 * KB, 44 * KB)
    WA = Arena(60 * KB, 32 * KB)
    B1 = Arena(92 * KB, 64 * KB)
    X2A = Arena(156 * KB, 64 * KB)
    PS = [nc.alloc_psum_tensor(f"ps{i}", [128, 512], F32).ap() for i in range(8)]
    WB = [WA(f"wb{i}", [128, 16, 256], BF16) for i in range(4)]
    cnt = {"w": 0, "p": 0, "t": 0, "o": 0, "e": 0}

    def nxt(key, n):
        v = cnt[key] % n
        cnt[key] += 1
        return v

    def dbg(name, ap):
        if name in dbg_names:
            shp = list(ap.shape)
            o = nc.dram_tensor("dbg_" + name, shp, ap.dtype if hasattr(ap, "dtype") else F32, kind="ExternalOutput").ap()
            k.dma(o, ap, eng=SP)
            dbg_out.append("dbg_" + name)

    def finish():
        k.emit(final_wait_tensors=["out"] + dbg_out if stop >= 12 else dbg_out)
        return nc, used_inputs, dbg_out

    identF = PA("identF", [128, 128]); k.dma(identF, din("identF", [128, 128]))
    identB = PA("identB", [128, 128], BF16); k.copy(identB, identF)
    cT = PA("cT", [128, 16]); k.dma(cT, din("cT", [128, 16]))
    na = PA("na", [128, 16]); k.dma(na, din("na", [128, 16]))
    nm_ = PA("nm", [128, 16]); k.dma(nm_, din("nm", [128, 16]))
    cols = PA("cols", [128, 6, 16])
    A1 = PA("A1", [128, 16]); A2 = PA("A2", [128, 16])
    gAB = PA("gAB", [128, 2048]); gMB = PA("gMB", [128, 2048])
    gB = {2: gAB, 5: gMB}
    fgsb = PA("fgsb", [128, 16, 32])
    small = PA("small", [128, 64])
    ss, rs, rstd = small[:, 0:1], small[:, 1:2], small[:, 2:3]
    ssq, rq, rstdq = small[:, 4:6], small[:, 6:8], small[:, 8:10]
    rc_, fac_ = small[:, 10:12], small[:, 12:14]

    X2A.reset(); B1.reset()
    TA = Arena(PA.base + 35 * KB, 9 * KB)
    sc_bf = TA("sc_bf", [128, 16], BF16)
    scB = TA("scB", [128, 16, 128], BF16)
    k.act(sc_bf, cT, AF.Silu)
    for kt in range(16):
        k.copy(scB[:, kt, :], sc_bf[:, kt:kt + 1].to_broadcast([128, 128]))
    modw = din("modw", [2048, 12288]).rearrange("(kt p) n -> p kt n", p=128)
    modbB = din("modbB", [128, 12288])
    mbs = [TA(f"mb{i}", [128, 256]) for i in range(2)]
    tmp2 = TA("tmp2", [128, 256]); tmp3 = TA("tmp3", [128, 128])

    def mod_chunk(ch):
        v, sub = ch // 8, ch % 8
        wb = WB[nxt("w", 4)]
        k.dma(wb, modw[:, :, ch * 256:(ch + 1) * 256], eng=POOL)
        mb = mbs[ch % 2]
        k.dma(mb, modbB[:, ch * 256:(ch + 1) * 256])
        ps = PS[nxt("p", 4)][:, 0:256]
        for kt in range(16):
            k.mm(ps, scB[:, kt, :], wb[:, kt, :], start=(kt == 0), stop=(kt == 15))
        if v in gB:
            k.tt(gB[v][:, sub * 256:(sub + 1) * 256], ps, mb, ALU.add)
        else:
            k.tt(tmp2, ps, mb, ALU.add)
            for jj in range(2):
                kt = sub * 2 + jj
                k.tt(tmp3, tmp2[:, jj * 128:(jj + 1) * 128], identF, ALU.mult)
                k.reduce_sum(cols[:, v, kt:kt + 1], tmp3)

    for ch in range(16):
        mod_chunk(ch)
    k.ts(A1, cols[:, 1, :], 1.0, None, ALU.add); k.tt(A1, A1, na, ALU.mult)
    Bs1, Bs2 = cols[:, 0, :], cols[:, 3, :]

    hT = B1("hT", [128, 16, 2048], BF16)
    xts = [X2A(f"xt{i}", [128, 2048]) for i in range(2)]
    junk = X2A("junk", [128, 2048], BF16)
    xns = [X2A(f"xn{i}", [128, 2048], BF16) for i in range(2)]
    xloc = din("xloc", [2048, 2048])

    def norm_transpose(src, dstT, col0, Acol, Bcol, m):
        ss_, rs_, rstd_ = small[:, 16 + 3 * m:17 + 3 * m], small[:, 17 + 3 * m:18 + 3 * m], small[:, 18 + 3 * m:19 + 3 * m]
        xn = xns[m % 2]
        k.act(junk, src, AF.Square, accum_out=ss_)
        k.act(rs_, ss_, AF.Sqrt, bias=EPS, scale=1.0 / D)
        k.recip(rstd_, rs_)
        k.ts(xn, src, rstd_, None, ALU.mult)
        for k4 in range(4):
            pt = PS[4 + nxt("t", 2)]
            for j in range(4):
                kt = k4 * 4 + j
                k.mm(pt[:, j * 128:(j + 1) * 128], xn[:, kt * 128:(kt + 1) * 128], identB)
            for j in range(4):
                kt = k4 * 4 + j
                if j % 2 == 0:
                    k.act(dstT[:, kt, col0:col0 + 128], pt[:, j * 128:(j + 1) * 128], AF.Identity, bias=Bcol[:, kt:kt + 1], scale=Acol[:, kt:kt + 1])
                else:
                    k.ts(dstT[:, kt, col0:col0 + 128], pt[:, j * 128:(j + 1) * 128], Acol[:, kt:kt + 1], Bcol[:, kt:kt + 1], ALU.mult, ALU.add)

    for m in range(16):
        xt = xts[m % 2]
        k.dma(xt, xloc[m * 128:(m + 1) * 128, :])
        norm_transpose(xt, hT, m * 128, A1, Bs1, m)
        mod_chunk(16 + 2 * m)
        mod_chunk(17 + 2 * m)
    k.ts(A2, cols[:, 4, :], 1.0, None, ALU.add); k.tt(A2, A2, nm_, ALU.mult)
    dbg("A1", A1); dbg("gAB", gAB)
    dbg("hT", hT[:, 0, :])
    if stop <= 2:
        return finish()

    k.barrier()
    X2A.reset()
    KTs = dscr("KTs", [12, 128, 2048]); KCr = dscr("KCr", [4, 128, 2048])
    Vs = dscr("Vs", [2048, 12, 128]); QTs = dscr("QTs", [16, 128, 1024])
    gt = X2A("gt", [128, 8, 128])
    gt_in = din("gt", [128, 6, 128])
    k.dma(gt[:, 0:6, :], gt_in)
    k.ts(gt[:, 6, :], gt[:, 0, :], SCALE, None, ALU.mult)
    k.ts(gt[:, 7, :], gt[:, 4, :], SCALE, None, ALU.mult)
    stgs = [X2A(f"stg{i}", [128, 256], BF16) for i in range(4)]
    st2s = [X2A(f"st2{i}", [128, 2, 128], BF16) for i in range(4)]
    junkh = X2A("junkh", [128, 128], BF16)
    winp = din("winp", [2048, NCH * 256]).rearrange("(kt p) n -> p kt n", p=128)
    pend = [None]

    def stage_c():
        if pend[0] is None:
            return
        kind, arg, m, stg = pend[0]
        pend[0] = None
        pt = PS[4 + nxt("t", 2)]
        for j in range(2):
            k.mm(pt[:, j * 128:(j + 1) * 128], stg[:, j * 128:(j + 1) * 128], identB)
        st2 = st2s[nxt("o", 4)]
        k.copy(st2, pt[:, 0:256].rearrange("p (j t) -> p j t", j=2))
        if kind == "T":
            dst = KCr[arg:arg + 2, :, m * 128:(m + 1) * 128]
        elif arg[1] == "K":
            dst = KTs[arg[2]:arg[2] + 2, :, m * 128:(m + 1) * 128]
        else:
            dst = QTs[arg[2]:arg[2] + 2, :, (m - 8) * 128:(m - 7) * 128]
        k.dma(dst.rearrange("j d t -> d j t"), st2, grp="s3")

    for ci, (kind, arg) in enumerate(CHUNKS):
        wb = WB[nxt("w", 4)]
        k.dma(wb, winp[:, :, ci * 256:(ci + 1) * 256], eng=POOL)
        tiles = range(16) if ci < 15 else range(8, 16)
        for m in tiles:
            ps = PS[nxt("p", 4)][:, 0:256]
            for kt in range(16):
                k.mm(ps, hT[:, kt, m * 128:(m + 1) * 128], wb[:, kt, :], start=(kt == 0), stop=(kt == 15))
            stage_c()
            if kind == "F":
                k.copy(fgsb[:, m, :], ps[:, 0:32], eng=ACT)
                continue
            stg = stgs[nxt("e", 4)]
            if kind == "V":
                k.copy(stg, ps, eng=ACT)
                k.dma(Vs[m * 128:(m + 1) * 128, arg:arg + 2, :], stg.rearrange("p (j d) -> p j d", j=2), grp="s3")
                continue
            if kind == "T":
                k.copy(stg, ps, eng=ACT)
            else:
                gi = arg[0]
                for j in range(2):
                    k.act(junkh, ps[:, j * 128:(j + 1) * 128], AF.Square, accum_out=ssq[:, j:j + 1])
                k.act(rq, ssq, AF.Sqrt, bias=EPS, scale=1.0 / HD)
                k.recip(rstdq, rq)
                for j in range(2):
                    k.stt(stg[:, j * 128:(j + 1) * 128], ps[:, j * 128:(j + 1) * 128], rstdq[:, j:j + 1], gt[:, gi, :], ALU.mult, ALU.mult)
            pend[0] = (kind, arg, m, stg)
    stage_c()
    if "QTs" in dbg_names:
        qd = X2A("qd", [128, 1024], BF16); k.dma(qd, QTs[0]); dbg("QTs", qd)
        kd = X2A("kd", [128, 2048], BF16); k.dma(kd, KTs[4]); dbg("KTs", kd)
    dbg("fgsb", fgsb)
    if stop <= 3:
        return finish()

    k.barrier()
    X2A.reset(); B1.reset()
    def cload(ar, name, shape, dt=F32, eng=SP, src=None):
        t = ar(name, shape, dt)
        k.dma(t, src if src is not None else din(name, shape), eng=(POOL if dt == BF16 else eng))
        return t
    triF = cload(X2A, "triF", [128, 128]); onesF = cload(X2A, "onesF", [128, 128]); e64 = cload(X2A, "e64", [128, 128])
    fbias = cload(X2A, "fbias", [128, 8]); gbias = cload(X2A, "gbias", [128, 24])
    padk8 = cload(PA, "padk8", [128, 16, 8]); farb = cload(X2A, "farb", [128, 8])
    padn = cload(PA, "padn", [128, 1])
    padfar = PA("padfar", [128, 16, 8]); biasF = PA("biasF", [128, 8, 16, 8]); gsig = PA("gsig", [128, 8, 24])
    fl = X2A("fl", [128, 16, 8]); sg_ = X2A("sg_", [128, 128]); ls = X2A("ls", [128, 128])
    wsb = X2A("wsb", [128, 128]); tot = X2A("tot", [128, 16, 8]); offs = X2A("offs", [128, 16, 8])
    cum = X2A("cum", [128, 16, 8]); crefs = X2A("crefs", [128, 16, 8]); cumpad = X2A("cumpad", [128, 16, 8])
    for m in range(16):
        k.tt(fl[:, m, :], fgsb[:, m, 0:8], fbias, ALU.add)
        k.tt(padfar[:, m, :], padk8[:, m, :], farb, ALU.add)
    flf = fl.rearrange("p m h -> p (m h)")
    k.act(sg_, flf, AF.Sigmoid); k.act(ls, sg_, AF.Ln)
    k.mm(PS[0][:, 0:128], triF, ls); k.mm(PS[1][:, 0:128], onesF, ls)
    k.copy(wsb, PS[0][:, 0:128]); k.copy(tot.rearrange("p m h -> p (m h)"), PS[1][:, 0:128])
    k.memset(offs[:, 0, :], 0.0)
    for m in range(1, 16):
        k.tt(offs[:, m, :], offs[:, m - 1, :], tot[:, m - 1, :], ALU.add)
    cumf = cum.rearrange("p m h -> p (m h)")
    k.tt(cumf, wsb, offs.rearrange("p m h -> p (m h)"), ALU.add)
    k.mm(PS[2][:, 0:128], e64, cumf)
    k.copy(crefs.rearrange("p m h -> p (m h)"), PS[2][:, 0:128])
    k.tt(cumpad, cum, padk8, ALU.subtract)
    for i in range(8):
        for kb in range(9 + i):
            k.tt(biasF[:, i, kb, :], crefs[:, 8 + i, :], cumpad[:, kb, :], ALU.subtract)
        k.tt(gsig[:, i, :], fgsb[:, 8 + i, 8:32], gbias, ALU.add)
    k.act(gsig.rearrange("p i g -> p (i g)"), gsig.rearrange("p i g -> p (i g)"), AF.Sigmoid)
    dbg("biasF", biasF.rearrange("p i k h -> p (i k h)")); dbg("gsig", gsig.rearrange("p i g -> p (i g)"))
    if stop <= 4:
        return finish()

    k.barrier()
    X2A.reset()
    oacc = X2A("oacc", [128, 8, 1024])
    B1.reset()
    AT = Arena(B1.base, 32 * KB)
    mixT = Arena(B1.base + 32 * KB, 32 * KB)("mixT", [128, 16, 1024], BF16)
    XT = Arena(X2A.base + 32 * KB, 32 * KB)
    gtc = XT("gtc", [128, 6, 128]); k.dma(gtc, gt_in)
    KCT = [PA(f"KCT{g}", [128, 127], BF16) for g in range(2)]
    VCa = [PA(f"VCa{g}", [128, 161], BF16) for g in range(2)]
    ovl_in = din("ovl", [127, 32])
    for g in range(2):
        k.memset(VCa[g][:, 128:129], 1.0)
        k.dma(VCa[g][0:127, 129:161], ovl_in, eng=POOL)
    posT = cload(XT, "posT", [128, 2, 32])
    w1_in = din("w1", [2, 4096, 256]); w2_in = din("w2", [2, 256, 128])
    w1sb = AT("w1sb", [128, 32, 256], BF16); w2sb = AT("w2sb", [128, 2, 128], BF16)
    kcraw = AT("kcraw", [128, 2048], BF16); tmpl = AT("tmpl", [128, 32, 127], BF16)
    GT = [AT(f"GT{i}", [128, 127], BF16) for i in range(2)]
    gx2 = XT("gx2", [128, 127]); gu = XT("gu", [128, 127]); gth = XT("gth", [128, 127])
    kcn = XT("kcn", [128, 128], BF16); junkc = XT("junkc", [128, 128], BF16)
    for kv in range(2):
        k.dma(w1sb, w1_in[kv].rearrange("(l d) h -> d l h", d=128), eng=POOL)
        k.dma(w2sb, w2_in[kv].rearrange("(hh p) d -> p hh d", p=128), eng=POOL)
        for g in range(2):
            k.dma(kcraw, KCr[kv * 2 + g])
            for l in range(32):
                k.ts(tmpl[:, l, :], kcraw[:, l:l + 2017:16], posT[:, kv, l:l + 1], None, ALU.add)
            for hh in range(2):
                ps = PS[hh][:, 0:127]
                for l in range(32):
                    k.mm(ps, w1sb[:, l, hh * 128:(hh + 1) * 128], tmpl[:, l, :], start=(l == 0), stop=(l == 31))
                k.act(gx2, ps, AF.Square)
                k.ts(gu, gx2, 0.044715, 1.0, ALU.mult, ALU.add)
                k.tt(gu, gu, ps, ALU.mult)
                k.act(gth, gu, AF.Tanh, scale=0.7978845608028654)
                k.ts(gth, gth, 0.5, 0.5, ALU.mult, ALU.add)
                k.tt(GT[hh], gth, ps, ALU.mult)
            pk = PS[2][0:127, 0:128]
            k.mm(pk, GT[0], w2sb[:, 0, :], start=True, stop=False)
            k.mm(pk, GT[1], w2sb[:, 1, :], start=False, stop=True)
            if kv == 0:
                k.act(junkc[0:127, :], pk, AF.Square, accum_out=ss[0:127, :])
                k.act(rs[0:127, :], ss[0:127, :], AF.Sqrt, bias=EPS, scale=1.0 / HD)
                k.recip(rstd[0:127, :], rs[0:127, :])
                k.stt(kcn[0:127, :], pk, rstd[0:127, :], gtc[0:127, 1, :], ALU.mult, ALU.mult)
                k.mm(PS[4][:, 0:127], kcn[0:127, :], identB[0:127, 0:127])
                k.copy(KCT[g], PS[4][:, 0:127])
            else:
                k.copy(VCa[g][0:127, 0:128], pk)
    dbg("KCT0", KCT[0]); dbg("VCa0", VCa[0])
    if stop <= 5:
        return finish()

    k.barrier()
    AT.reset(); XT.reset()
    qTs = [AT(f"qT{i}", [128, 1024], BF16) for i in range(2)]
    bcs = [AT(f"bc{i}", [128, 1024], BF16) for i in range(2)]
    pTs = [AT(f"pT{i}", [128, 512], BF16) for i in range(4)]
    vAs = [AT(f"vA{i}", [128, 16, 129], BF16) for i in range(2)]
    for v_ in vAs:
        k.memset(v_[:, :, 128:129], 1.0)
    negselT = [AT(f"negsel{g}", [32, 1024], BF16) for g in range(2)]
    t01 = cload(AT, "t01", [128, 8, 2, 128], BF16)
    wm4 = cload(AT, "wm4", [128, 128], BF16); causalB = cload(AT, "causal", [128, 128], BF16)
    imp = XT("imp", [128, 8, 32]); vmul = cload(XT, "vmul", [128, 8, 32]); amask = cload(XT, "amask", [128, 8, 32])
    impm = XT("impm", [128, 32]); wk_ = XT("wk_", [128, 32]); m8 = XT("m8", [128, 16]); nsel = XT("nsel", [128, 32], BF16)
    onormB = cload(XT, "onormB", [128, 2048])
    kTs = [XT(f"kT{i}", [128, 2048], BF16) for i in range(2)]
    ekb = cload(XT, "ekb", [32, 16, 128], BF16)
    mix = XT("mix", [128, 1024], BF16); junko = XT("junko", [128, 1024], BF16)
    biascmp_in = din("biascmp", [8, 127, 1024])
    qcnt = [0]

    def load_q(hq):
        q = qTs[qcnt[0] % 2]; qcnt[0] += 1
        k.dma(q, QTs[hq])
        return q

    for g in range(2):
        for r in range(4):
            h = 4 * g + r
            qT = load_q(h)
            bc = bcs[h % 2]
            k.dma(bc[0:127, :], biascmp_in[h], eng=POOL)
            for half in range(2):
                ps = PS[nxt("p", 4)][0:127, :]
                k.mm(ps, KCT[g], qT[:, half * 512:(half + 1) * 512], start=True, stop=False)
                k.mm(ps, identB[0:127, 0:127], bc[0:127, half * 512:(half + 1) * 512], start=False, stop=True)
                pT = pTs[nxt("e", 4)]
                k.act(pT[0:127, :], ps, AF.Exp, bias=padn[0:127, :])
                for i4 in range(4):
                    i = half * 4 + i4
                    po = PS[6 + nxt("o", 2)][:, 0:161]
                    k.mm(po, pT[0:127, i4 * 128:(i4 + 1) * 128], VCa[g][0:127, :])
                    k.ts(rc_[:, 1:2], po[:, 128:129], 1e-30, None, ALU.max)
                    k.recip(rc_[:, 0:1], rc_[:, 1:2])
                    k.tt(fac_[:, 0:1], rc_[:, 0:1], gsig[:, i, h:h + 1], ALU.mult)
                    k.ts(oacc[:, i, h * 128:(h + 1) * 128], po[:, 0:128], fac_[:, 0:1], None, ALU.mult)
                    if r == 0:
                        k.ts(imp[:, i, :], po[:, 129:161], rc_[:, 0:1], None, ALU.mult)
                    else:
                        k.stt(imp[:, i, :], po[:, 129:161], rc_[:, 0:1], imp[:, i, :], ALU.mult, ALU.add)
        for i in range(8):
            k.tt(impm, imp[:, i, :], vmul[:, i, :], ALU.mult)
            k.tt(impm, impm, amask[:, i, :], ALU.add)
            k.max8(m8[:, 0:8], impm)
            k.match_replace(wk_, m8[:, 0:8], impm, -1e9)
            k.max8(m8[:, 8:16], wk_)
            k.ts(nsel, impm, m8[:, 15:16], -NEG, ALU.is_ge, ALU.mult)
            k.ts(nsel, nsel, NEG, None, ALU.add)
            k.mm(PS[4][0:32, 0:128], nsel, identB)
            k.copy(negselT[g][:, i * 128:(i + 1) * 128], PS[4][0:32, 0:128])
    dbg("oacc_cmp", oacc[:, 0, :]); dbg("negsel0", negselT[0])
    if stop <= 6:
        return finish()

    kvc = [0]

    def load_kv(idx):
        kT = kTs[kvc[0] % 2]; vA = vAs[kvc[0] % 2]; kvc[0] += 1
        k.dma(kT, KTs[idx])
        k.dma(vA[:, :, 0:128], Vs[:, idx, :].rearrange("(m p) d -> p m d", p=128))
        return kT, vA

    def attend(kT, vA, qT, h, i, kbs, mode, g):
        qb = 8 + i
        po = PS[6 + nxt("o", 2)][:, 0:129]

        def qk(kb):
            dl = qb - kb
            ps = PS[nxt("p", 4)][:, 0:128]
            extra = []
            if mode == "fox":
                if dl == 0:
                    extra.append((identB, causalB))
                bias = biasF[:, i, kb, h:h + 1]
            else:
                if dl <= 1:
                    extra.append((identB, t01[:, h, dl, :]))
                    bias = padk8[:, kb, 0:1]
                else:
                    bias = padfar[:, kb, h:h + 1]
                    if mode == "win" and dl == 4:
                        extra.append((identB, wm4))
                if mode == "slc":
                    extra.append((ekb[:, kb, :], negselT[g][:, i * 128:(i + 1) * 128]))
            k.mm(ps, kT[:, kb * 128:(kb + 1) * 128], qT[:, i * 128:(i + 1) * 128], start=True, stop=(len(extra) == 0))
            for xi, (l_, r_) in enumerate(extra):
                k.mm(ps, l_, r_, start=False, stop=(xi == len(extra) - 1))
            return ps, bias

        pend = [qk(kb) for kb in kbs[:2]]
        for idx, kb in enumerate(kbs):
            if idx + 2 < len(kbs):
                pend.append(qk(kbs[idx + 2]))
            ps, bias = pend.pop(0)
            pT = pTs[nxt("e", 4)][:, 0:128]
            k.act(pT, ps, AF.Exp, bias=bias)
            k.mm(po, pT, vA[:, kb, :], start=(idx == 0), stop=(idx == len(kbs) - 1))
        return po

    for g in range(2):
        for mode, kidx, goff in (("slc", g, 8), ("win", 2 + g, 16)):
            kT, vA = load_kv(kidx)
            for r in range(4):
                h = 4 * g + r
                qT = load_q(h)
                for i in range(8):
                    qb = 8 + i
                    kbs = list(range(qb + 1)) if mode == "slc" else list(range(qb - 4, qb + 1))
                    po = attend(kT, vA, qT, h, i, kbs, mode, g)
                    k.recip(rc_[:, 0:1], po[:, 128:129])
                    k.tt(fac_[:, 0:1], rc_[:, 0:1], gsig[:, i, goff + h:goff + h + 1], ALU.mult)
                    k.stt(oacc[:, i, h * 128:(h + 1) * 128], po[:, 0:128], fac_[:, 0:1], oacc[:, i, h * 128:(h + 1) * 128], ALU.mult, ALU.add)

    def out_norm(koff):
        for i in range(8):
            k.act(junko, oacc[:, i, :], AF.Square, accum_out=ss)
            k.act(rs, ss, AF.Sqrt, bias=EPS, scale=1.0 / 1024)
            k.recip(rstd, rs)
            k.stt(mix, oacc[:, i, :], rstd, onormB[:, koff * 128:koff * 128 + 1024], ALU.mult, ALU.mult)
            for k4 in range(2):
                pt = PS[4 + nxt("t", 2)]
                for j in range(4):
                    kt = k4 * 4 + j
                    k.mm(pt[:, j * 128:(j + 1) * 128], mix[:, kt * 128:(kt + 1) * 128], identB)
                k.copy(mixT[:, koff + k4 * 4:koff + k4 * 4 + 4, i * 128:(i + 1) * 128], pt.rearrange("p (j t) -> p j t", j=4), eng=(ACT if k4 == 0 else DVE))

    dbg("oacc_nsa", oacc[:, 0, :])
    out_norm(0)
    if stop <= 7:
        dbg("mixT", mixT[:, 0, :])
        return finish()
    for h in range(8):
        kT, vA = load_kv(4 + h)
        qT = load_q(8 + h)
        for i in range(8):
            po = attend(kT, vA, qT, h, i, list(range(9 + i)), "fox", 0)
            k.recip(rc_[:, 0:1], po[:, 128:129])
            k.ts(oacc[:, i, h * 128:(h + 1) * 128], po[:, 0:128], rc_[:, 0:1], None, ALU.mult)
    dbg("oacc_fox", oacc[:, 0, :])
    out_norm(8)
    dbg("mixT", mixT[:, 8, :])
    if stop <= 8:
        return finish()

    k.barrier()
    X2A.reset(); AT.reset()
    x2 = X2A("x2", [128, 8, 2048])
    for i in range(8):
        k.dma(x2[:, i, :], xloc[(8 + i) * 128:(9 + i) * 128, :])
    tmps = [AT(f"tmpo{i}", [128, 512]) for i in range(3)]
    wout = din("wout", [2048, 2048]).rearrange("(kt p) n -> p kt n", p=128)
    for c in range(8):
        wb = WB[nxt("w", 4)]
        k.dma(wb, wout[:, :, c * 256:(c + 1) * 256], eng=POOL)
        for i in range(8):
            ps = PS[nxt("p", 4)][:, 0:256]
            for kt in range(16):
                k.mm(ps, mixT[:, kt, i * 128:(i + 1) * 128], wb[:, kt, :], start=(kt == 0), stop=(kt == 15))
            tmp = tmps[nxt("e", 3)][:, 0:256]
            k.tt(tmp, ps, gAB[:, c * 256:(c + 1) * 256], ALU.mult)
            k.tt(x2[:, i, c * 256:(c + 1) * 256], x2[:, i, c * 256:(c + 1) * 256], tmp, ALU.add)
    dbg("x2", x2[:, 0, :])
    if stop <= 9:
        return finish()

    k.barrier()
    B1.reset()
    h2T = B1("h2T", [128, 16, 1024], BF16)
    actT = B1("actT", [128, 16, 1024], BF16)
    MT = Arena(B1.base + 32 * KB, 32 * KB)
    junk2 = MT("junk2", [128, 2048], BF16); xn2 = MT("xn2", [128, 2048], BF16)
    junk, xn = junk2, xn2
    def norm_transpose2(src, dstT, col0, Acol, Bcol):
        k.act(junk2, src, AF.Square, accum_out=ss)
        k.act(rs, ss, AF.Sqrt, bias=EPS, scale=1.0 / D)
        k.recip(rstd, rs)
        k.ts(xn2, src, rstd, None, ALU.mult)
        for k4 in range(4):
            pt = PS[4 + nxt("t", 2)]
            for j in range(4):
                kt = k4 * 4 + j
                k.mm(pt[:, j * 128:(j + 1) * 128], xn2[:, kt * 128:(kt + 1) * 128], identB)
            for j in range(4):
                kt = k4 * 4 + j
                if j % 2 == 0:
                    k.act(dstT[:, kt, col0:col0 + 128], pt[:, j * 128:(j + 1) * 128], AF.Identity, bias=Bcol[:, kt:kt + 1], scale=Acol[:, kt:kt + 1])
                else:
                    k.ts(dstT[:, kt, col0:col0 + 128], pt[:, j * 128:(j + 1) * 128], Acol[:, kt:kt + 1], Bcol[:, kt:kt + 1], ALU.mult, ALU.add)
    for i in range(8):
        norm_transpose2(x2[:, i, :], h2T, i * 128, A2, Bs2)
    rwsb = MT("rwsb", [128, 16, 32], BF16)
    k.dma(rwsb, din("rw", [2048, 32]).rearrange("(kt p) e -> p kt e", p=128), eng=POOL)
    rbB = cload(MT, "rbB", [128, 32])
    gates = PA("gates", [128, 8, 32]); bupT = cload(PA, "bupT", [128, 32, 32])
    lg = MT("lg", [128, 32]); m8r = MT("m8r", [128, 8]); nmx = MT("nmx", [128, 1]); msk = MT("msk", [128, 32])
    ex = MT("ex", [128, 32]); rsm = MT("rsm", [128, 1]); rrm = MT("rrm", [128, 1])
    gTs = MT("gTs", [32, 128]); bdn = cload(MT, "bdn", [32, 2048])
    tmpb = [MT(f"tmpb{i}", [128, 512]) for i in range(2)]
    for i in range(8):
        ps = PS[nxt("p", 4)][:, 0:32]
        for kt in range(16):
            k.mm(ps, h2T[:, kt, i * 128:(i + 1) * 128], rwsb[:, kt, :], start=(kt == 0), stop=(kt == 15))
        k.tt(lg, ps, rbB, ALU.add)
        k.max8(m8r, lg)
        k.ts(nmx, m8r[:, 0:1], -1.0, None, ALU.mult)
        k.ts(msk, lg, m8r[:, 3:4], None, ALU.is_ge)
        k.act(ex, lg, AF.Exp, bias=nmx)
        k.tt(ex, ex, msk, ALU.mult)
        k.reduce_sum(rsm, ex)
        k.recip(rrm, rsm)
        k.ts(gates[:, i, :], ex, rrm, None, ALU.mult)
        k.mm(PS[4][0:32, 0:128], gates[:, i, :], identF)
        k.copy(gTs, PS[4][0:32, 0:128])
        for c in range(4):
            pb = PS[nxt("p", 4)]
            k.mm(pb, gTs, bdn[:, c * 512:(c + 1) * 512])
            tb = tmpb[c % 2]
            k.tt(tb, pb, gMB[:, c * 512:(c + 1) * 512], ALU.mult)
            k.tt(x2[:, i, c * 512:(c + 1) * 512], x2[:, i, c * 512:(c + 1) * 512], tb, ALU.add)
    dbg("gates", gates.rearrange("p i e -> p (i e)")); dbg("h2T", h2T[:, 0, :])
    if stop <= 10:
        return finish()

    k.barrier()
    ET = Arena(PA.base + PA.off, PA.size - PA.off)
    tg = ET("tg", [128, 512]); tsg = ET("tsg", [128, 512]); tl = ET("tl", [128, 512]); ta = ET("ta", [128, 512])
    tds = [ET(f"td{i}", [128, 256]) for i in range(2)]
    wup = din("wup", [NEXP, 2048, 4096]); wdn = din("wdn", [NEXP, 2048, 2048])
    pset = [0]
    for e in range(nexp):
        wu = wup[e].rearrange("(kt p) f -> p kt f", p=128)
        for c in range(8):
            wg = WB[nxt("w", 4)]; k.dma(wg, wu[:, :, c * 256:(c + 1) * 256], eng=POOL)
            wl = WB[nxt("w", 4)]; k.dma(wl, wu[:, :, 2048 + c * 256:2048 + (c + 1) * 256], eng=POOL)
            for j in range(2):
                ft = c * 2 + j
                base = 4 * (pset[0] % 2); pset[0] += 1
                for kt in range(16):
                    for tc in range(2):
                        k.mm(PS[base + tc], wg[:, kt, j * 128:(j + 1) * 128], h2T[:, kt, tc * 512:(tc + 1) * 512], start=(kt == 0), stop=(kt == 15))
                for kt in range(16):
                    for tc in range(2):
                        k.mm(PS[base + 2 + tc], wl[:, kt, j * 128:(j + 1) * 128], h2T[:, kt, tc * 512:(tc + 1) * 512], start=(kt == 0), stop=(kt == 15))
                for tc in range(2):
                    k.ts(tg, PS[base + tc], bupT[:, e, ft:ft + 1], 7.0, ALU.add, ALU.min)
                    k.act(tsg, tg, AF.Sigmoid, scale=1.702)
                    k.ts(tl, PS[base + 2 + tc], bupT[:, e, 16 + ft:17 + ft], 7.0, ALU.add, ALU.min)
                    k.ts(tl, tl, -7.0, 1.0, ALU.max, ALU.add)
                    k.tt(ta, tg, tsg, ALU.mult)
                    k.tt(actT[:, ft, tc * 512:(tc + 1) * 512], ta, tl, ALU.mult)
        wd_ = wdn[e].rearrange("(kt p) d -> p kt d", p=128)
        for c in range(8):
            wd = WB[nxt("w", 4)]; k.dma(wd, wd_[:, :, c * 256:(c + 1) * 256], eng=POOL)
            for i in range(8):
                ps = PS[nxt("p", 8)][:, 0:256]
                for kt in range(16):
                    k.mm(ps, actT[:, kt, i * 128:(i + 1) * 128], wd[:, kt, :], start=(kt == 0), stop=(kt == 15))
                td = tds[nxt("e", 2)]
                k.tt(td, ps, gMB[:, c * 256:(c + 1) * 256], ALU.mult)
                k.stt(x2[:, i, c * 256:(c + 1) * 256], td, gates[:, i, e:e + 1], x2[:, i, c * 256:(c + 1) * 256], ALU.mult, ALU.add)

    outd = nc.dram_tensor("out", [1024, 2048], F32, kind="ExternalOutput").ap()
    for i in range(8):
        k.dma(outd[i * 128:(i + 1) * 128, :], x2[:, i, :])
    stop = 12
    return finish()


def _t5_bucket(dist):
    n = np.maximum(dist, 0)
    nf = np.maximum(n, 1).astype(np.float32)
    large = 16 + (np.log(nf / np.float32(16)) / np.float32(math.log(8.0)) * np.float32(16)).astype(np.int32)
    large = np.minimum(large, 31)
    return np.where(n < 16, n, large)


def host_prep(inp, core):
    f32 = np.float32
    b, p = core // 2, core % 2
    x = np.asarray(inp["x"], f32)
    o = {}
    if p == 1:
        o["xloc"] = np.ascontiguousarray(x[b])
    else:
        o["xloc"] = np.concatenate([np.zeros((1024, D), f32), x[b, :1024]], axis=0)
    col = lambda v: np.ascontiguousarray(np.asarray(v, f32).reshape(16, 128).T)
    rep = lambda v: np.ascontiguousarray(np.broadcast_to(np.asarray(v, f32).reshape(1, -1), (128, np.asarray(v).size)))
    o["cT"] = col(inp["c"][b])
    o["na"] = col(inp["norm_attn"][0]); o["nm"] = col(inp["norm_moe"][0])
    o["modw"] = np.asarray(inp["mod_w"][0], f32)
    o["modbB"] = rep(inp["mod_b"][0])
    o["identF"] = np.eye(128, dtype=f32)
    w = np.asarray(inp["w_in"][0], f32)
    sizes = [1024, 256, 256, 256, 256, 256, 256, 24, 1024, 1024, 1024, 8]
    offs = np.concatenate([[0], np.cumsum(sizes)])
    seg = lambda i: w[:, offs[i]:offs[i + 1]]
    fcols = np.zeros((D, 256), f32)
    fcols[:, 0:8] = seg(11); fcols[:, 8:32] = seg(7)
    o["winp"] = np.ascontiguousarray(np.concatenate(
        [seg(1), seg(2), seg(3), seg(5), seg(4), seg(6), seg(9), seg(10), fcols, seg(0), seg(8)], axis=1))
    kn = np.asarray(inp["nsa_k_norm"][0], f32)
    gts = [inp["nsa_q_norm"][0], kn[0], kn[1], kn[2], inp["fox_q_norm"][0], inp["fox_k_norm"][0]]
    o["gt"] = np.ascontiguousarray(np.stack([rep(g) for g in gts], axis=1))
    o["fbias"] = rep(inp["fox_forget_bias"][0]); o["gbias"] = rep(inp["b_nsa_gate"][0])
    o["posT"] = np.ascontiguousarray(np.stack([np.asarray(inp["cmp_pos_k"][0], f32).T, np.asarray(inp["cmp_pos_v"][0], f32).T], axis=1))
    o["w1"] = np.ascontiguousarray(np.stack([inp["cmp_k_w1"][0], inp["cmp_v_w1"][0]]).astype(f32))
    o["w2"] = np.ascontiguousarray(np.stack([inp["cmp_k_w2"][0], inp["cmp_v_w2"][0]]).astype(f32))
    n = np.arange(127)[:, None]; j = np.arange(32)[None, :]
    o["ovl"] = ((16 * n < 64 * j + 64) & (16 * n + 32 > 64 * j)).astype(f32)
    rb = np.asarray(inp["rel_bias"], f32)
    t = np.arange(1024)[None, :]
    d = (1024 + t) - (16 * n + 31)
    bc = rb[_t5_bucket(d)]
    bc = np.where((d >= 0)[..., None], bc, f32(NEG))
    o["biascmp"] = np.ascontiguousarray(bc.transpose(2, 0, 1).astype(f32))
    s = np.arange(128)[:, None]; tt_ = np.arange(128)[None, :]
    t01 = np.zeros((128, 8, 2, 128), f32)
    for dl in range(2):
        dd = dl * 128 + tt_ - s
        v = rb[_t5_bucket(dd)]
        v = np.where((dd >= 0)[..., None], v, f32(NEG))
        t01[:, :, dl, :] = v.transpose(0, 2, 1)
    o["t01"] = t01
    o["farb"] = rep(rb[31])
    o["wm4"] = np.where(s > tt_, f32(0), f32(NEG)).astype(f32)
    o["causal"] = np.where(s <= tt_, f32(0), f32(NEG)).astype(f32)
    sl = np.arange(128)[:, None] + 128 * np.arange(16)[None, :]
    padk = np.where((sl < 1024) & (p == 0), f32(NEG), f32(0)).astype(f32)
    o["padk8"] = np.ascontiguousarray(np.broadcast_to(padk[:, :, None], (128, 16, 8)))
    pn = np.zeros((128, 1), f32)
    if p == 0:
        pn[:64] = NEG
    o["padn"] = pn
    tl = (1024 + np.arange(1024)).reshape(8, 128).T
    treal = tl - 1024 * (1 - p)
    cur = (treal // 64)[:, :, None]
    jr = (np.arange(32) - 16 * (1 - p))[None, None, :]
    valid = (jr >= 0) & (jr <= cur)
    f0 = valid & (jr == 0); f1 = valid & (jr == cur) & ~f0; f2 = valid & (jr == cur - 1) & ~f0 & ~f1
    am = np.where(f0, 3e6, np.where(f1, 2e6, np.where(f2, 1e6, np.where(valid, 0.0, -1e6))))
    o["amask"] = am.astype(f32)
    o["vmul"] = (valid & ~f0 & ~f1 & ~f2).astype(f32)
    ek = np.zeros((32, 16, 128), f32)
    for kb in range(16):
        ek[2 * kb, kb, :64] = 1; ek[2 * kb + 1, kb, 64:] = 1
    o["ekb"] = ek
    o["triF"] = (np.arange(128)[:, None] <= np.arange(128)[None, :]).astype(f32)
    o["onesF"] = np.ones((128, 128), f32)
    e64 = np.zeros((128, 128), f32); e64[64, :] = 1
    o["e64"] = e64
    o["onormB"] = rep(inp["out_norm"][0])
    o["wout"] = np.asarray(inp["w_out"][0], f32)
    o["rw"] = np.asarray(inp["router_w"][0], f32)
    o["rbB"] = rep(inp["router_b"][0])
    o["wup"] = np.asarray(inp["exp_w_up"][0], f32)
    o["wdn"] = np.asarray(inp["exp_w_down"][0], f32)
    bu = np.asarray(inp["exp_b_up"][0], f32)
    o["bupT"] = np.ascontiguousarray(bu.reshape(32, 32, 128).transpose(2, 0, 1))
    o["bdn"] = np.asarray(inp["exp_b_down"][0], f32)
    return o


_CACHE = {}


def kernel(**inputs):
    if "prog" not in _CACHE:
        _CACHE["prog"] = build()
    nc, used, _ = _CACHE["prog"]
    in_maps = []
    for core in range(8):
        hp = host_prep(inputs, core)
        in_maps.append({n: hp[n] for n in used})
    res = run_bass_kernel_spmd(nc, in_maps, core_ids=list(range(8)))
    out = np.zeros((4, 2048, D), np.float32)
    for core in range(8):
        b, p = core // 2, core % 2
        out[b, p * 1024:(p + 1) * 1024] = res.results[core]["out"]
    return out
```
